# Optimizing a Trainium2 kernel written in Bass

```python
import math
import jax
import jax.numpy as jnp
from jax import lax
import numpy as np

D_MODEL = 1024
BATCH = 2
SEQ = 8192
DEPTH = 1

GRID_W = 64
CTX_LEN = 256
EPS = 1e-6

MIX_WIDTH = D_MODEL
SSD_INNER = MIX_WIDTH // 2
SSD_HEAD_DIM = 64
SSD_HEADS = SSD_INNER // SSD_HEAD_DIM
SSD_GROUPS = 2
SSD_HEADS_PER_GROUP = SSD_HEADS // SSD_GROUPS
SSD_STATE = 128
CONV_WIDTH = 5
CONV_DIM = SSD_INNER + 2 * SSD_GROUPS * SSD_STATE
CHUNK = 128
DT_MIN = 0.001
DT_MAX = 0.1
ATTN_WIDTH = MIX_WIDTH - SSD_INNER
HEAD_DIM = 64
ATTN_Q_HEADS = ATTN_WIDTH // HEAD_DIM
ATTN_KV_HEADS = 2
Q_PER_KV = ATTN_Q_HEADS // ATTN_KV_HEADS
KV_WIDTH = ATTN_KV_HEADS * HEAD_DIM
WINDOW = 128
BLOCK = 128
ROPE_BASE = 10000.0
IN_COLS = SSD_INNER + CONV_DIM + 2 * SSD_HEADS + ATTN_WIDTH + 2 * KV_WIDTH
N_GROUPS = 4
EXPERTS_PER_GROUP = 8
N_EXPERTS = N_GROUPS * EXPERTS_PER_GROUP
TOP_K = 2
EXPERT_DIM = D_MODEL // 2
MOE_BLOCK = 128

kernel_name = 'hymba_ssd_swa_hmoe_prefix_dit'


def rms_norm(x, gain):
    xf = x.astype(jnp.float32)
    y = xf * lax.rsqrt(jnp.mean(xf * xf, axis=-1, keepdims=True) + EPS)
    return (y * gain).astype(x.dtype)


def _flip(t):
    return jnp.flip(t, axis=1)


def split_projection(p):
    sizes = [SSD_INNER, CONV_DIM, 2 * SSD_HEADS, ATTN_WIDTH, KV_WIDTH, KV_WIDTH]
    return jnp.split(p, np.cumsum(sizes)[:-1].tolist(), axis=-1)


def centred_dwconv(u, w, b):
    out = lax.conv_general_dilated(
        u, w[:, None, :].astype(u.dtype), window_strides=(1,),
        padding=((CONV_WIDTH // 2, CONV_WIDTH // 2),),
        dimension_numbers=('NWC', 'WIO', 'NWC'), feature_group_count=u.shape[-1])
    return out + b.astype(u.dtype)


def ssd_inputs(xbc, dt_raw, conv_w, conv_b, dt_bias):
    b, L = xbc.shape[:2]
    u = jax.nn.silu(centred_dwconv(xbc, conv_w, conv_b))
    xs, bm, cm = jnp.split(u, [SSD_INNER, SSD_INNER + SSD_GROUPS * SSD_STATE], axis=-1)
    xs = xs.reshape(b, L, SSD_HEADS, SSD_HEAD_DIM)
    bm = bm.reshape(b, L, SSD_GROUPS, SSD_STATE)
    cm = cm.reshape(b, L, SSD_GROUPS, SSD_STATE)
    dt = jax.nn.softplus(dt_raw.astype(jnp.float32).reshape(b, L, 2, SSD_HEADS) + dt_bias)
    return xs, bm, cm, dt


def ssd_scan(xs, dt, a, bm, cm, h0, need_y=True):
    f32 = jnp.float32
    b, L = xs.shape[:2]
    nc = L // CHUNK
    G, R, P, N = SSD_GROUPS, SSD_HEADS_PER_GROUP, SSD_HEAD_DIM, SSD_STATE
    xg = xs.astype(f32).reshape(b, nc, CHUNK, G, R, P)
    dtc = dt.astype(f32).reshape(b, nc, CHUNK, G, R)
    bc = bm.astype(f32).reshape(b, nc, CHUNK, G, N)
    a_cs = jnp.cumsum(dtc * a.reshape(G, R), axis=2)
    a_last = a_cs[:, :, -1]
    dtx = dtc[..., None] * xg
    states = jnp.einsum('bcjgn,bcjgr,bcjgrp->bcgrpn', bc, jnp.exp(a_last[:, :, None] - a_cs), dtx)

    def step(h, inp):
        s, al = inp
        return jnp.exp(al)[..., None, None] * h + s, h

    h_final, h_prev = lax.scan(step, h0.astype(f32),
                               (jnp.moveaxis(states, 1, 0), jnp.moveaxis(a_last, 1, 0)))
    if not need_y:
        return None, h_final
    cc = cm.astype(f32).reshape(b, nc, CHUNK, G, N)
    tri = jnp.tril(jnp.ones((CHUNK, CHUNK), dtype=bool))
    seg = a_cs[:, :, :, None] - a_cs[:, :, None, :]
    decay = jnp.exp(jnp.where(tri[:, :, None, None], seg, -jnp.inf))
    cb = jnp.einsum('bcign,bcjgn->bcijg', cc, bc)
    y_diag = jnp.einsum('bcijgr,bcjgrp->bcigrp', cb[..., None] * decay, dtx)
    y_off = jnp.einsum('bcign,cbgrpn->bcigrp', cc, h_prev) * jnp.exp(a_cs)[..., None]
    return (y_diag + y_off).reshape(b, L, SSD_HEADS, P), h_final


def ssd_output(y, xs, z, d_skip, gain):
    b, L = xs.shape[:2]
    y = y + d_skip.astype(jnp.float32)[:, None] * xs.astype(jnp.float32)
    y = y.reshape(b, L, SSD_INNER) * jax.nn.silu(z.astype(jnp.float32))
    return rms_norm(y, gain).astype(z.dtype)


def axial_rope_tables(rows):
    row = jnp.repeat(jnp.arange(rows), GRID_W)
    col = jnp.tile(jnp.arange(GRID_W), rows)
    n_freq = HEAD_DIM // 4
    inv_freq = ROPE_BASE ** (-jnp.arange(n_freq, dtype=jnp.float32) / n_freq)
    ang = jnp.concatenate([row[:, None] * inv_freq, col[:, None] * inv_freq], axis=-1)
    ang = jnp.concatenate([ang, ang], axis=-1)
    return jnp.cos(ang), jnp.sin(ang)


def apply_rope(t, cos, sin):
    half = HEAD_DIM // 2
    rot = jnp.concatenate([-t[..., half:], t[..., :half]], axis=-1)
    return (t * cos[:, None] + rot * sin[:, None]).astype(t.dtype)


def softmax_with_sink(logits, sink):
    full = jnp.concatenate([logits, jnp.broadcast_to(sink, logits.shape[:-1] + (1,))], axis=-1)
    return jax.nn.softmax(full, axis=-1)[..., :-1]


def windowed_attention(q, k, v, k_ctx, v_ctx, sinks):
    f32 = jnp.float32
    b, L = q.shape[:2]
    nb = L // BLOCK
    scale = HEAD_DIM ** -0.5
    qb = q.reshape(b, nb, BLOCK, ATTN_KV_HEADS, Q_PER_KV, HEAD_DIM)

    def band(t):
        tp = jnp.pad(t, ((0, 0), (BLOCK, BLOCK), (0, 0), (0, 0)))
        tp = tp.reshape(b, nb + 2, BLOCK, ATTN_KV_HEADS, HEAD_DIM)
        return jnp.concatenate([tp[:, :-2], tp[:, 1:-1], tp[:, 2:]], axis=2)

    kw, vw = band(k), band(v)
    s_loc = jnp.einsum('bnqhgd,bnkhd->bnhgqk', qb, kw, preferred_element_type=f32) * scale
    blk = jnp.arange(nb)[:, None, None] * BLOCK
    q_pos = blk + jnp.arange(BLOCK)[None, :, None]
    k_pos = blk + jnp.arange(3 * BLOCK)[None, None, :] - BLOCK
    valid = (jnp.abs(k_pos - q_pos) <= WINDOW) & (k_pos >= 0) & (k_pos < L)
    s_loc = jnp.where(valid[None, :, None, None], s_loc, -jnp.inf)
    s_ctx = jnp.einsum('bnqhgd,bkhd->bnhgqk', qb, k_ctx, preferred_element_type=f32) * scale
    sink = sinks.astype(f32).reshape(ATTN_KV_HEADS, Q_PER_KV)[:, :, None, None]
    p = softmax_with_sink(jnp.concatenate([s_loc, s_ctx], axis=-1), sink)
    p_loc = p[..., :3 * BLOCK].astype(v.dtype)
    p_ctx = p[..., 3 * BLOCK:].astype(v.dtype)
    o = (jnp.einsum('bnhgqk,bnkhd->bnqhgd', p_loc, vw)
         + jnp.einsum('bnhgqk,bkhd->bnqhgd', p_ctx, v_ctx))
    return o.reshape(b, L, ATTN_WIDTH)


def context_attention(q_c, k_c, v_c, sinks):
    b, Lc = q_c.shape[:2]
    qg = q_c.reshape(b, Lc, ATTN_KV_HEADS, Q_PER_KV, HEAD_DIM)
    s = jnp.einsum('bqhgd,bkhd->bhgqk', qg, k_c, preferred_element_type=jnp.float32) * HEAD_DIM ** -0.5
    sink = sinks.astype(jnp.float32).reshape(ATTN_KV_HEADS, Q_PER_KV)[:, :, None, None]
    p = softmax_with_sink(s, sink).astype(v_c.dtype)
    return jnp.einsum('bhgqk,bkhd->bqhgd', p, v_c).reshape(b, Lc, ATTN_WIDTH)


def hier_moe(h, w_group, b_group, w_expert, b_expert, w_gate, w_up, w_down):
    f32 = jnp.float32
    n_tok, d = h.shape
    g_prob = jax.nn.softmax((h @ w_group).astype(f32) + b_group, axis=-1)
    g_w, g_idx = lax.top_k(g_prob, 1)
    e_logits = ((h @ w_expert).astype(f32) + b_expert).reshape(n_tok, N_GROUPS, EXPERTS_PER_GROUP)
    e_sel = e_logits[jnp.arange(n_tok), g_idx[:, 0]]
    e_w, e_idx = lax.top_k(jax.nn.softmax(e_sel, axis=-1), TOP_K)
    weights = g_w * e_w / jnp.sum(e_w, axis=-1, keepdims=True)
    expert = g_idx * EXPERTS_PER_GROUP + e_idx
    n_assign = n_tok * TOP_K
    flat_e = expert.reshape(n_assign).astype(jnp.int32)
    flat_tok = jnp.repeat(jnp.arange(n_tok, dtype=jnp.int32), TOP_K)
    flat_w = weights.reshape(n_assign)
    order = jnp.argsort(flat_e)
    sorted_e = flat_e[order]
    counts = jnp.zeros((N_EXPERTS,), jnp.int32).at[flat_e].add(1)
    start = jnp.cumsum(counts) - counts
    padded = (counts + MOE_BLOCK - 1) // MOE_BLOCK * MOE_BLOCK
    ends = jnp.cumsum(padded)
    pstart = ends - padded
    dest = pstart[sorted_e] + jnp.arange(n_assign, dtype=jnp.int32) - start[sorted_e]
    n_blocks = -(-n_assign // MOE_BLOCK) + N_EXPERTS
    n_slots = n_blocks * MOE_BLOCK
    tok_buf = jnp.full((n_slots,), n_tok, jnp.int32).at[dest].set(flat_tok[order])
    w_buf = jnp.zeros((n_slots,), f32).at[dest].set(flat_w[order])
    h_pad = jnp.concatenate([h, jnp.zeros((1, d), h.dtype)], axis=0)
    x_buf = h_pad[tok_buf].reshape(n_blocks, MOE_BLOCK, d)
    blk_e = jnp.searchsorted(ends, jnp.arange(n_blocks, dtype=jnp.int32) * MOE_BLOCK, side='right')
    blk_e = jnp.minimum(blk_e, N_EXPERTS - 1)

    def expert_block(args):
        xb, e = args
        return (jax.nn.silu(xb @ w_gate[e]) * (xb @ w_up[e])) @ w_down[e]

    y_buf = lax.map(expert_block, (x_buf, blk_e)).reshape(n_slots, d)
    out = jax.ops.segment_sum(y_buf.astype(f32) * w_buf[:, None], tok_buf, num_segments=n_tok + 1)
    return out[:n_tok].astype(h.dtype)


def setup_inputs(seed: int = 0) -> dict:
    key = jax.random.key(seed)
    ks = jax.random.split(key, 26)
    f32 = jnp.float32

    def nrm(k, shape, scale):
        return jax.random.normal(k, shape, f32) * scale

    u = jax.random.uniform(ks[10], (DEPTH, 2, SSD_HEADS), f32)
    dt0 = jnp.exp(u * (math.log(DT_MAX) - math.log(DT_MIN)) + math.log(DT_MIN))
    return {
        'x': nrm(ks[0], (BATCH, SEQ, D_MODEL), 1.0),
        'c': nrm(ks[1], (BATCH, D_MODEL), 1.0),
        'ctx': nrm(ks[2], (BATCH, CTX_LEN, D_MODEL), 1.0),
        'c_ctx': nrm(ks[3], (D_MODEL,), 1.0),
        'w_ada': nrm(ks[4], (DEPTH, D_MODEL, 6 * D_MODEL), 0.5 * D_MODEL ** -0.5),
        'b_ada': nrm(ks[5], (DEPTH, 6 * D_MODEL), 0.02),
        'norm1': 1.0 + nrm(ks[6], (DEPTH, D_MODEL), 0.02),
        'w_in': nrm(ks[7], (DEPTH, D_MODEL, IN_COLS), D_MODEL ** -0.5),
        'conv_w': nrm(ks[8], (DEPTH, CONV_WIDTH, CONV_DIM), CONV_WIDTH ** -0.5),
        'conv_b': nrm(ks[9], (DEPTH, CONV_DIM), 0.02),
        'dt_bias': dt0 + jnp.log(-jnp.expm1(-dt0)),
        'a_log': jnp.log(jax.random.uniform(ks[11], (DEPTH, 2, SSD_HEADS), f32, 1.0, 16.0)),
        'd_skip': 1.0 + nrm(ks[12], (DEPTH, SSD_HEADS), 0.1),
        'ssd_norm': 1.0 + nrm(ks[13], (DEPTH, SSD_INNER), 0.02),
        'attn_sinks': nrm(ks[14], (DEPTH, ATTN_Q_HEADS), 1.0),
        'w_out': nrm(ks[15], (DEPTH, MIX_WIDTH, D_MODEL), MIX_WIDTH ** -0.5),
        'norm2': 1.0 + nrm(ks[16], (DEPTH, D_MODEL), 0.02),
        'w_group': nrm(ks[17], (DEPTH, D_MODEL, N_GROUPS), D_MODEL ** -0.5),
        'b_group': nrm(ks[18], (DEPTH, N_GROUPS), 0.01),
        'w_expert': nrm(ks[19], (DEPTH, D_MODEL, N_EXPERTS), D_MODEL ** -0.5),
        'b_expert': nrm(ks[20], (DEPTH, N_EXPERTS), 0.01),
        'w_gate': nrm(ks[21], (DEPTH, N_EXPERTS, D_MODEL, EXPERT_DIM), D_MODEL ** -0.5),
        'w_up': nrm(ks[22], (DEPTH, N_EXPERTS, D_MODEL, EXPERT_DIM), D_MODEL ** -0.5),
        'w_down': nrm(ks[23], (DEPTH, N_EXPERTS, EXPERT_DIM, D_MODEL), EXPERT_DIM ** -0.5),
        'norm_final': 1.0 + nrm(ks[24], (D_MODEL,), 0.02),
    }


def reference(x, c, ctx, c_ctx, w_ada, b_ada, norm1, w_in, conv_w, conv_b, dt_bias, a_log,
              d_skip, ssd_norm, attn_sinks, w_out, norm2, w_group, b_group, w_expert, b_expert,
              w_gate, w_up, w_down, norm_final):
    b, n_tok, d = x.shape
    n_ctx = ctx.shape[1]
    rows = n_tok // GRID_W
    cos, sin = axial_rope_tables(rows)
    h0 = jnp.zeros((b, SSD_GROUPS, SSD_HEADS_PER_GROUP, SSD_HEAD_DIM, SSD_STATE), jnp.float32)
    for layer in range(DEPTH):
        last = layer == DEPTH - 1
        mod_x = jax.nn.silu(c) @ w_ada[layer] + b_ada[layer]
        mod_c = jax.nn.silu(c_ctx) @ w_ada[layer] + b_ada[layer]
        sh1, sc1, g1, sh2, sc2, g2 = jnp.split(mod_x[:, None, :], 6, axis=-1)
        csh1, csc1, cg1, csh2, csc2, cg2 = jnp.split(mod_c, 6, axis=-1)
        hx = rms_norm(x, norm1[layer]) * (1 + sc1) + sh1
        hc = rms_norm(ctx, norm1[layer]) * (1 + csc1) + csh1
        zx, xbc_x, dt_x, qx, kx, vx = split_projection(hx @ w_in[layer])
        zc, xbc_c, dt_c, qc, kc, vc = split_projection(hc @ w_in[layer])
        xs_x, bm_x, cm_x, dtv_x = ssd_inputs(xbc_x, dt_x, conv_w[layer], conv_b[layer], dt_bias[layer])
        xs_c, bm_c, cm_c, dtv_c = ssd_inputs(xbc_c, dt_c, conv_w[layer], conv_b[layer], dt_bias[layer])
        A = -jnp.exp(a_log[layer].astype(jnp.float32))
        y_cf, h_cf = ssd_scan(xs_c, dtv_c[:, :, 0], A[0], bm_c, cm_c, h0, need_y=not last)
        y_cb, h_cb = ssd_scan(_flip(xs_c), _flip(dtv_c[:, :, 1]), A[1], _flip(bm_c), _flip(cm_c), h0,
                              need_y=not last)
        y_xf, _ = ssd_scan(xs_x, dtv_x[:, :, 0], A[0], bm_x, cm_x, h_cf)
        y_xb, _ = ssd_scan(_flip(xs_x), _flip(dtv_x[:, :, 1]), A[1], _flip(bm_x), _flip(cm_x), h_cb)
        ssd_x = ssd_output(y_xf + _flip(y_xb), xs_x, zx, d_skip[layer], ssd_norm[layer])
        qx = apply_rope(qx.reshape(b, n_tok, ATTN_Q_HEADS, HEAD_DIM), cos, sin)
        kx = apply_rope(kx.reshape(b, n_tok, ATTN_KV_HEADS, HEAD_DIM), cos, sin)
        vx = vx.reshape(b, n_tok, ATTN_KV_HEADS, HEAD_DIM)
        kc = kc.reshape(b, n_ctx, ATTN_KV_HEADS, HEAD_DIM)
        vc = vc.reshape(b, n_ctx, ATTN_KV_HEADS, HEAD_DIM)
        attn_x = windowed_attention(qx, kx, vx, kc, vc, attn_sinks[layer])
        x = x + g1 * (jnp.concatenate([ssd_x, attn_x], axis=-1) @ w_out[layer])
        h2 = rms_norm(x, norm2[layer]) * (1 + sc2) + sh2
        moe_x = hier_moe(h2.reshape(b * n_tok, d), w_group[layer], b_group[layer], w_expert[layer],
                         b_expert[layer], w_gate[layer], w_up[layer], w_down[layer])
        x = x + g2 * moe_x.reshape(b, n_tok, d)
        if not last:
            ssd_c = ssd_output(y_cf + _flip(y_cb), xs_c, zc, d_skip[layer], ssd_norm[layer])
            attn_c = context_attention(qc.reshape(b, n_ctx, ATTN_Q_HEADS, HEAD_DIM), kc, vc, attn_sinks[layer])
            ctx = ctx + cg1 * (jnp.concatenate([ssd_c, attn_c], axis=-1) @ w_out[layer])
            h2c = rms_norm(ctx, norm2[layer]) * (1 + csc2) + csh2
            moe_c = hier_moe(h2c.reshape(b * n_ctx, d), w_group[layer], b_group[layer], w_expert[layer],
                             b_expert[layer], w_gate[layer], w_up[layer], w_down[layer])
            ctx = ctx + cg2 * moe_c.reshape(b, n_ctx, d)
    return rms_norm(x, norm_final)
```

```python
from contextlib import ExitStack
import numpy as np
import ml_dtypes
import concourse.bass as bass
import concourse.mybir as mybir
from concourse.bass_utils import run_bass_kernel_spmd

F32 = mybir.dt.float32
BF16 = mybir.dt.bfloat16
AF = mybir.ActivationFunctionType
ALU = mybir.AluOpType
AX = mybir.AxisListType

NCORES = 8
D = 1024
T = 2048
NT = 16
NE = 32
EPS = 1e-6
NEG = -30000.0


class _Op:
    __slots__ = ("eng", "fn", "deps", "waits", "needed", "count", "dsem", "idx")


class Sched:
    def __init__(self, nc, es):
        self.nc, self.es = nc, es
        self.ops = []
        self.last_w, self.readers = {}, {}
        self.dma_sems, self.eng_sem = {}, {}
        self.alias = {}
        self.keyname = {}
        self.alias_done = set()

    def _sem(self, name):
        return self.es.enter_context(self.nc.semaphore(name))

    def op(self, eng, fn, reads=(), writes=(), dsem=None):
        o = _Op()
        o.eng, o.fn, o.dsem, o.needed, o.count, o.idx = eng, fn, dsem, False, None, len(self.ops)
        writes = list(writes)
        for k in list(writes):
            if k in self.alias_done:
                continue
            self.alias_done.add(k)
            for nm in (k, k[0] if isinstance(k, tuple) else None, self.keyname.get(k)):
                if nm is not None and nm in self.alias:
                    writes.extend(self.alias[nm])
        deps = set()
        for k in reads:
            w = self.last_w.get(k)
            if w is not None:
                deps.add(w)
        for k in writes:
            w = self.last_w.get(k)
            if w is not None and not (w.dsem is not None and dsem is not None):
                deps.add(w)
            deps.update(self.readers.get(k, ()))
        deps.discard(o)
        if eng == "tensor" and dsem is None:
            deps = {d for d in deps if not (d.eng == "tensor" and d.dsem is None)}
        o.deps = deps
        for k in writes:
            self.last_w[k] = o
            self.readers[k] = []
        for k in reads:
            self.readers.setdefault(k, []).append(o)
        self.ops.append(o)
        return o

    def finalize(self):
        for o in self.ops:
            for d in o.deps:
                d.needed = True
        cnt = {}
        for o in self.ops:
            if o.dsem is not None:
                if o.dsem not in self.dma_sems:
                    self.dma_sems[o.dsem] = self._sem("d_" + str(o.dsem))
                cnt[("d", o.dsem)] = cnt.get(("d", o.dsem), 0) + (1 if str(o.dsem).startswith("C:") else 16)
                o.count = cnt[("d", o.dsem)]
            elif o.needed:
                if o.eng not in self.eng_sem:
                    self.eng_sem[o.eng] = self._sem("e_" + o.eng)
                cnt[o.eng] = cnt.get(o.eng, 0) + 1
                o.count = cnt[o.eng]
        for o in self.ops:
            if o.dsem is not None and isinstance(o.dsem, str) and o.dsem.startswith("G:"):
                o.count = cnt[("d", o.dsem)]
        waited = {}
        for o in self.ops:
            need = {}
            for d in o.deps:
                sk = ("d", d.dsem) if d.dsem is not None else d.eng
                if d.count > need.get(sk, 0):
                    need[sk] = d.count
            o.waits = []
            for sk, v in need.items():
                if waited.get((o.eng, sk), 0) >= v:
                    continue
                waited[(o.eng, sk)] = v
                o.waits.append((sk, v))
        self.final_counts = cnt

    def emit(self, final_wait_eng="sync"):
        with self.nc.Block() as block:
            for eng in ("sync", "gpsimd", "scalar", "vector", "tensor"):
                ops = [o for o in self.ops if o.eng == eng]
                last = eng == final_wait_eng

                def body(e, ops=ops, last=last):
                    for o in ops:
                        for sk, v in o.waits:
                            s = self.dma_sems[sk[1]] if isinstance(sk, tuple) else self.eng_sem[sk]
                            e.wait_ge(s, v)
                        inst = o.fn(e)
                        if o.dsem is not None and str(o.dsem).startswith("C:"):
                            inst.then_inc(self.dma_sems[o.dsem])
                        elif o.dsem is not None:
                            inst.then_inc(self.dma_sems[o.dsem], 16)
                        elif o.needed:
                            inst.then_inc(self.eng_sem[o.eng], 1)
                    if last:
                        for sk, v in self.final_counts.items():
                            s = self.dma_sems[sk[1]] if isinstance(sk, tuple) else self.eng_sem[sk]
                            e.wait_ge(s, v)

                getattr(block, eng)(body)


def build_nc(debug=(), stop=None):
    nc = bass.Bass("TRN2", target_bir_lowering=False)

    def din(name, shape, dt=F32):
        return nc.dram_tensor(name, list(shape), dt, kind="ExternalInput").ap()

    xe = din("xe", [2304, D])
    ctxb = din("ctxb", [256, D])
    cfm = din("cfm", [128, 16])
    vmask = din("vmask", [128, 4])
    mvec = din("mvec", [128, 8])
    amask_d = din("amask", [128, 4 * 512], BF16)
    rope_d = din("rope", [128, 2 * 2304], BF16)
    wfm = din("wfm", [20, 128, 8 * 128])
    wvd_d = din("wvd", [128, 8 * 144])
    wz_d = din("wz", [128, 8 * 512])
    w_ada = din("w_ada", [D, 6 * D])
    b_ada = din("b_ada", [6 * D])
    norm1 = din("norm1", [D])
    convw_d = din("convw", [128, 40])
    convb_d = din("convb", [128, 8])
    dtb_d = din("dtb", [16])
    alog_d = din("alog", [16])
    dskip_d = din("dskip", [8])
    ssdn_d = din("ssdnorm", [512])
    sinks_d = din("sinks", [8])
    w_out = din("w_out", [D, D])
    norm2 = din("norm2", [D])
    wr_d = din("wr", [128, 8 * 36])
    br_d = din("br", [36])
    ne_decl = NE if stop is None else 1
    w_gate = din("w_gate", [ne_decl, D, 512])
    w_up = din("w_up", [ne_decl, D, 512])
    w_down = din("w_down", [ne_decl, 512, D])
    normf = din("normf", [D])
    out = nc.dram_tensor("out", [T, D], F32, kind="ExternalOutput").ap()
    xin = nc.dram_tensor("xin", [128, 1040], F32)
    xout = nc.dram_tensor("xout", [4 * 128, 1040], F32)
    CAP = 512
    Xbuf = nc.dram_tensor("Xbuf", [NE * CAP, D], BF16)
    Ybuf = nc.dram_tensor("Ybuf", [NE * CAP, D], BF16)
    dbg = {}
    for name, shape, dt_ in debug:
        dbg[name] = nc.dram_tensor("dbg_" + name, list(shape), dt_, kind="ExternalOutput").ap()

    es = ExitStack()
    with es:
        S = Sched(nc, es)
        ARENA_W = 52736
        arena = es.enter_context(nc.sbuf_tensor("arena", [128, ARENA_W], F32))
        banks = [es.enter_context(nc.psum_tensor(f"ps{i}", [128, 512], F32)) for i in range(8)]

        def PB(i):
            return banks[i][:, :]

        def PBh(i):
            return banks[i][:, :].bitcast(BF16)

        class Region:
            def __init__(self, base_kib, size_kib):
                self.base = int(base_kib * 256)
                self.size = int(size_kib * 256)
                self.off = 0

            def reset(self):
                self.off = 0

            def get(self, nelem, dt=F32, name=None):
                words = (nelem * (2 if dt == BF16 else 4) + 3) // 4
                words = (words + 7) // 8 * 8
                a = self.base + self.off
                self.off += words
                assert self.off <= self.size, (name, self.off, self.size)
                if name is not None:
                    olds = [k for (lo, hi, k) in ALLOCS if lo < a + words and a < hi and k != name]
                    if olds:
                        S.alias.setdefault(name, [])
                        for k in olds:
                            S.alias[name].extend(ALLKEYS.get(k, [k]))
                    ALLOCS.append((a, a + words, name))
                    S.alias_done = {k for k in S.alias_done
                                    if not (k == name or (isinstance(k, tuple) and k[0] == name) or S.keyname.get(k) == name)}
                ap = arena[:, a:a + words]
                if dt == BF16:
                    ap = ap.bitcast(BF16)
                return ap[:, 0:nelem]

        ALLOCS = []
        ALLKEYS = {}

        def keys(name, n):
            ks = [(name, i) for i in range(n)]
            ALLKEYS[name] = ks
            return ks

        R_const = Region(0, 4)
        R_vec = Region(4, 6)
        R_mod2 = Region(10, 12)
        R_A = Region(22, 32)
        R_B = Region(54, 16)
        R_C = Region(70, 16)
        R_hx = Region(86, 40)
        R_mod1 = Region(126, 20)
        R_dt = Region(146, 8)
        R_BC = Region(154, 17)
        R_tm = Region(171, 27)
        R_x = Region(154, 44)
        R_late = Region(126, 72)
        R_misc = Region(198, 8)

        def dma(eng, sem, out_, in_, r=(), w=()):
            S.op(eng, lambda e: e.dma_start(out=out_, in_=in_), r, w, dsem=sem)

        def tt(eng, out_, in0, in1, op, r=(), w=()):
            S.op(eng, lambda e: e.tensor_tensor(out=out_, in0=in0, in1=in1, op=op), r, w)

        def ts(eng, out_, in0, s1, s2, op0, op1=None, r=(), w=()):
            if op1 is None:
                S.op(eng, lambda e: e.tensor_scalar(out=out_, in0=in0, scalar1=s1, scalar2=None, op0=op0), r, w)
            else:
                S.op(eng, lambda e: e.tensor_scalar(out=out_, in0=in0, scalar1=s1, scalar2=s2, op0=op0, op1=op1), r, w)

        def stt(out_, in0, sc, in1, op0, op1, r=(), w=()):
            S.op("vector", lambda e: e.scalar_tensor_tensor(out=out_, in0=in0, scalar=sc, in1=in1, op0=op0, op1=op1), r, w)

        def act(out_, in_, func, r=(), w=(), bias=None, scale=None, accum=None):
            kw = {}
            if bias is not None:
                kw["bias"] = bias
            if scale is not None:
                kw["scale"] = scale
            if accum is not None:
                kw["accum_out"] = accum
            S.op("scalar", lambda e: e.activation(out=out_, in_=in_, func=func, **kw), r, w)

        def mm(out_, lhsT, rhs, start, stop, r=(), w=()):
            S.op("tensor", lambda e: e.matmul(out_, lhsT=lhsT, rhs=rhs, start=start, stop=stop, skip_group_check=True), r, w)

        def tr(out_, in_, ident, r=(), w=()):
            S.op("tensor", lambda e: e.transpose(out=out_, in_=in_, identity=ident), r, w)

        def cp(eng, out_, in_, r=(), w=()):
            if eng == "scalar":
                S.op(eng, lambda e: e.copy(out=out_, in_=in_), r, w)
            else:
                S.op(eng, lambda e: e.tensor_copy(out=out_, in_=in_), r, w)

        def red(out_, in_, op, r=(), w=()):
            S.op("vector", lambda e: e.tensor_reduce(out=out_, in_=in_, axis=AX.X, op=op), r, w)

        def recip(out_, in_, r=(), w=()):
            S.op("vector", lambda e: e.reciprocal(out=out_, in_=in_), r, w)

        def mset(eng, ap, val, w=()):
            S.op(eng, lambda e: e.memset(ap, val), (), w)

        def asel(ap, pattern, cmp, base, cm, key):
            S.op("gpsimd", lambda e: e.affine_select(out=ap, in_=ap, pattern=pattern, compare_op=cmp, fill=0.0,
                                                     base=base, channel_multiplier=cm), [key], [key])

        def tap(name, src, r):
            if name in dbg:
                dma("sync", "dbg", dbg[name], src, r=r)

        def v3(ap, a):
            return ap.rearrange("p (a b) -> p a b", a=a)

        def bc(ap, shape):
            return ap.to_broadcast(list(shape))

        def finish():
            S.finalize()
            S.emit()

        identf = R_const.get(128)
        identb = R_const.get(128, BF16)
        LE = R_const.get(128)
        GE = R_const.get(128)
        GT = R_const.get(128)
        LT = R_const.get(128)
        onesf = R_const.get(128)
        mset("gpsimd", identf, 1.0, w=["identf"])
        asel(identf, [[-1, 128]], ALU.is_equal, 0, 1, "identf")
        cp("vector", identb, identf, r=["identf"], w=["identb"])
        mset("gpsimd", onesf, 1.0, w=["onesf"])
        for ap_, key, pat, cmpop, base, cm in (
            (LE, "LE", [[1, 128]], ALU.is_ge, 0, -1),
            (GE, "GE", [[-1, 128]], ALU.is_ge, 0, 1),
            (GT, "GT", [[-1, 128]], ALU.is_gt, 0, 1),
            (LT, "LT", [[1, 128]], ALU.is_gt, 0, -1),
        ):
            mset("gpsimd", ap_, 1.0, w=[key])
            asel(ap_, pat, cmpop, base, cm, key)

        zt = R_misc.get(1024, BF16)
        mset("gpsimd", zt, 0.0, w=["zt"])

        convw = R_vec.get(40)
        convb = R_vec.get(8)
        dtb = R_vec.get(16)
        aneg = R_vec.get(16)
        d8 = R_vec.get(8)
        D512 = R_vec.get(512)
        ssdn = R_vec.get(512)
        esink = R_vec.get(8)
        vm = R_vec.get(4)
        mv = R_vec.get(8)
        brb = R_vec.get(36)
        wr = R_vec.get(288)
        dma("gpsimd", "G:vec", convw, convw_d[:, :], w=["convw"])
        dma("gpsimd", "G:vec", convb, convb_d[:, :], w=["convb"])
        dma("gpsimd", "G:vec", dtb, dtb_d.partition_broadcast(128), w=["dtb"])
        dma("gpsimd", "G:vec", aneg, alog_d.partition_broadcast(128), w=["aneg"])
        dma("gpsimd", "G:vec", d8, dskip_d.partition_broadcast(128), w=["d8"])
        dma("gpsimd", "G:vec", ssdn, ssdn_d.partition_broadcast(128), w=["ssdn"])
        dma("gpsimd", "G:vec", esink, sinks_d.partition_broadcast(128), w=["esink"])
        dma("gpsimd", "G:vec", vm, vmask[:, :], w=["vm"])
        dma("gpsimd", "G:vec", mv, mvec[:, :], w=["mv"])
        dma("gpsimd", "G:vec", brb, br_d.partition_broadcast(128), w=["brb"])
        dma("gpsimd", "G:vec", wr, wr_d[:, :], w=["wr"])
        act(aneg, aneg, AF.Exp, r=["aneg"], w=["aneg"])
        ts("vector", aneg, aneg, -1.0, None, ALU.mult, r=["aneg"], w=["aneg"])
        act(esink, esink, AF.Exp, r=["esink"], w=["esink"])
        cp("vector", v3(D512, 8), bc(d8.unsqueeze(2), [128, 8, 64]), r=["d8"], w=["D512"])

        sh2 = R_mod2.get(D)
        gsc2 = R_mod2.get(D)
        g2 = R_mod2.get(D)
        sh1 = R_mod1.get(D, name="sh1")
        gsc1 = R_mod1.get(D, name="gsc1")
        g1 = R_mod1.get(D, name="g1")
        csh1 = R_mod1.get(D, name="csh1")
        cgsc1 = R_mod1.get(D, name="cgsc1")
        modx = [sh1, gsc1, g1, sh2, gsc2, g2]
        modc = [csh1, cgsc1]
        modx_k = ["sh1", "gsc1", "g1", "sh2", "gsc2", "g2"]
        modc_k = ["csh1", "cgsc1"]
        for k_ in modx_k + modc_k:
            keys(k_, 2)

        scf = R_tm.get(16, name="scf")
        Lrep = R_tm.get(16 * 128, name="Lrep")
        n1b = R_tm.get(D, name="n1b")
        n2b = R_tm.get(D, name="n2b")
        dma("sync", "scf", scf, cfm[:, :], w=["scf"])
        dma("sync", "G:nb", n1b, norm1.partition_broadcast(128), w=["n1b"])
        dma("sync", "G:nb", n2b, norm2.partition_broadcast(128), w=["n2b"])
        act(scf, scf, AF.Silu, r=["scf"], w=["scf"])
        cp("vector", v3(Lrep, 16), bc(scf.unsqueeze(2), [128, 16, 128]), r=["scf"], w=["Lrep"])
        Lr = v3(Lrep, 16)
        keys("wst", 2)
        keys("bst", 2)
        wst = [R_A.get(8 * 512, name="wst"), R_A.get(8 * 512, name="wst")]
        bst = [R_B.get(512, name="bst"), R_B.get(512, name="bst")]
        w_ada_v = w_ada.rearrange("(kc p) n -> p kc n", p=128)
        for j in range(12):
            sl = j % 2
            dma("sync", ("wst", sl), v3(wst[sl], 8), w_ada_v[:, :, j * 512:(j + 1) * 512], w=[("wst", sl)])
            dma("gpsimd", ("bst", sl), bst[sl], b_ada[j * 512:(j + 1) * 512].partition_broadcast(128), w=[("bst", sl)])
            variants = [(0, modx[j // 2], modx_k[j // 2], sl)]
            if j < 4:
                variants.append((1, modc[j // 2], modc_k[j // 2], 2 + sl))
            for vi, dst, dk, bank in variants:
                for kc in range(8):
                    mm(PB(bank), Lr[:, vi * 8 + kc, :], v3(wst[sl], 8)[:, kc, :], kc == 0, kc == 7,
                       r=["Lrep", ("wst", sl)], w=[("ps", bank)])
                tt("vector", dst[:, (j % 2) * 512:(j % 2 + 1) * 512], PB(bank), bst[sl], ALU.add,
                   r=[("ps", bank), ("bst", sl)], w=[(dk, j % 2)])
        for dst, dk, nb, nk in ((gsc1, "gsc1", n1b, "n1b"), (cgsc1, "cgsc1", n1b, "n1b"), (gsc2, "gsc2", n2b, "n2b")):
            stt(dst, dst, 1.0, nb, ALU.add, ALU.mult, r=[(dk, 0), (dk, 1), nk], w=[(dk, 0), (dk, 1)])
        tap("sh1", sh1, [("sh1", 0), ("sh1", 1)])
        tap("gsc1", gsc1, [("gsc1", 0), ("gsc1", 1)])
        tap("g2", g2, [("g2", 0), ("g2", 1)])

        keys("hxT", 20)
        hxT = v3(R_hx.get(8 * 2560, BF16, name="hxT"), 8)
        keys("xst", 3)
        xst = [R_C.get(D, name="xst") for _ in range(3)]
        keys("hxb", 2)
        hxb = [R_B.get(D, BF16, name="hxb") for _ in range(2)]
        junk = R_B.get(D, BF16, name="junk")
        hxf = R_B.get(D, name="hxf")
        ssq = R_dt.get(32)
        keys("ssq", 20)
        for t in range(20):
            sl = t % 3
            src = xe[t * 128:(t + 1) * 128, :] if t < 18 else ctxb[(t - 18) * 128:(t - 17) * 128, :]
            dma("sync", ("xst", sl), xst[sl], src, w=[("xst", sl)])
            act(junk, xst[sl], AF.Square, r=[("xst", sl)], w=["junk", ("ssq", t)], accum=ssq[:, t:t + 1])
            act(ssq[:, t:t + 1], ssq[:, t:t + 1], AF.Sqrt, r=[("ssq", t)], w=[("ssq", t)], bias=EPS, scale=1.0 / D)
            recip(ssq[:, t:t + 1], ssq[:, t:t + 1], r=[("ssq", t)], w=[("ssq", t)])
            gs, gk, sh, sk = (gsc1, "gsc1", sh1, "sh1") if t < 18 else (cgsc1, "cgsc1", csh1, "csh1")
            stt(hxf, xst[sl], ssq[:, t:t + 1], gs, ALU.mult, ALU.mult,
                r=[("xst", sl), ("ssq", t), (gk, 0), (gk, 1)], w=["hxf"])
            hs = t % 2
            tt("vector", hxb[hs], hxf, sh, ALU.add, r=["hxf", (sk, 0), (sk, 1)], w=[("hxb", hs)])
            if t in (0, 17):
                c = 0 if t == 0 else 1
                ts("vector", hxb[hs], hxb[hs], vm[:, c:c + 1], None, ALU.mult, r=[("hxb", hs), "vm"], w=[("hxb", hs)])
            bank = t % 2
            for kc in range(8):
                tr(PBh(bank)[:, kc * 128:(kc + 1) * 128], hxb[hs][:, kc * 128:(kc + 1) * 128], identb,
                   r=[("hxb", hs), "identb"], w=[("ps", bank)])
            cp("scalar", hxT[:, :, t * 128:(t + 1) * 128], v3(PBh(bank)[:, 0:1024], 8), r=[("ps", bank)], w=[("hxT", t)])
        for kc_ in range(8):
            tap("hxT%d" % kc_, hxT[:, kc_, :], [("hxT", t) for t in range(20)])
        if stop == "B":
            finish()
            return nc

        R_A.reset()
        qT = v3(R_A.get(4 * 2048, BF16, name="qT"), 4)
        kTd = v3(R_A.get(2 * 2304, BF16, name="kTd"), 2)
        vaug_f = R_A.get(20 * 2 * 66, BF16, name="vaug")
        vaug = vaug_f.rearrange("p (t h c) -> p t h c", t=20, h=2)
        kcT = v3(R_A.get(2 * 256, BF16, name="kcT"), 2)
        keys("qT", 16)
        keys("kTd", 18)
        keys("vaug", 20)
        keys("kcT", 2)
        amask = R_x.get(4 * 512, BF16, name="amask")
        ropeT = R_x.get(2 * 2304, BF16, name="ropeT")
        cosT, sinT = ropeT[:, 0:2304], ropeT[:, 2304:4608]
        keys("PT", 5)
        PT = [R_x.get(512, BF16, name="PT") for _ in range(5)]
        keys("rt", 4)
        rt = [R_x.get(512, name="rt") for _ in range(4)]
        keys("wsf", 2)
        wsf_all = R_x.get(2048, name="wsf")
        wsf = [wsf_all[:, 0:1024], wsf_all[:, 1024:2048]]
        keys("wsb", 4)
        wsb_all = R_x.get(4096, BF16, name="wsb")
        wsb = [wsb_all[:, i * 1024:(i + 1) * 1024] for i in range(4)]
        dtraw_f = R_misc.get(20 * 16)
        dtraw = v3(dtraw_f, 20)
        keys("dtraw", 20)
        dma("gpsimd", "amask", amask, amask_d[:, :], w=["amask"])
        dma("gpsimd", "ropeT", ropeT, rope_d[:, :], w=["ropeT"])
        mset("vector", dtraw_f, 0.0, w=keys("dtraw", 20))
        wvdf = wsf_all[:, 0:1152]
        wvdb = wsb_all[:, 0:1152]
        dma("sync", ("wsf", 0), wvdf, wvd_d[:, :], w=[("wsf", 0), ("wsf", 1)])
        cp("vector", wvdb, wvdf, r=[("wsf", 0), ("wsf", 1)], w=[("wsb", 0), ("wsb", 1)])
        mset("gpsimd", vaug_f, 1.0, w=keys("vaug", 20))
        for t in range(20):
            bank = 6 + t % 2
            for kc in range(8):
                mm(PB(bank)[:, 0:144], hxT[:, kc, t * 128:(t + 1) * 128], v3(wvdb, 8)[:, kc, :], kc == 0, kc == 7,
                   r=[("hxT", t), ("wsb", 0), ("wsb", 1)], w=[("ps", bank)])
            cp("scalar", vaug[:, t, :, 0:64], PB(bank)[:, 0:128].rearrange("p (a b) -> p a b", a=2),
               r=[("ps", bank)], w=[("vaug", t)])
            if 1 <= t <= 16 or t >= 18:
                cp("scalar", dtraw[:, t, :], PB(bank)[:, 128:144], r=[("ps", bank)], w=[("dtraw", t)])
        if stop == "C0":
            tap("vaug", vaug_f, [("vaug", i) for i in range(20)])
            finish()
            return nc
        wcnt = [0]

        def load_w(ci, slot_b):
            sf = wcnt[0] % 2
            wcnt[0] += 1
            dma("sync", ("wsf", sf), wsf[sf], wfm[ci], w=[("wsf", sf)])
            cp("gpsimd", wsb[slot_b], wsf[sf], r=[("wsf", sf)], w=[("wsb", slot_b)])

        gcnt = [0]

        def proj_pair(sa, sb, e0, n, dst, dkeys, do_rope=True):
            i = gcnt[0] % 2
            gcnt[0] += 1
            a, b_ = 2 + 2 * i, 3 + 2 * i
            hk_ = [("hxT", t) for t in range(e0 // 128, (e0 + n + 127) // 128)]
            for kc in range(8):
                mm(PB(a)[:, 0:n], v3(wsb[sa], 8)[:, kc, :], hxT[:, kc, e0:e0 + n], kc == 0, kc == 7,
                   r=hk_ + [("wsb", sa)], w=[("ps", a)])
            if not do_rope:
                cp("scalar", dst, PB(a)[:, 0:n], r=[("ps", a)], w=dkeys)
                return
            for kc in range(8):
                mm(PB(b_)[:, 0:n], v3(wsb[sb], 8)[:, kc, :], hxT[:, kc, e0:e0 + n], kc == 0, kc == 7,
                   r=hk_ + [("wsb", sb)], w=[("ps", b_)])
            tt("vector", rt[2 * i][:, 0:n], PB(a)[:, 0:n], cosT[:, e0:e0 + n], ALU.mult, r=[("ps", a), "ropeT"], w=[("rt", 2 * i)])
            tt("vector", rt[2 * i + 1][:, 0:n], PB(b_)[:, 0:n], sinT[:, e0:e0 + n], ALU.mult, r=[("ps", b_), "ropeT"], w=[("rt", 2 * i + 1)])
            tt("gpsimd", dst, rt[2 * i][:, 0:n], rt[2 * i + 1][:, 0:n], ALU.add, r=[("rt", 2 * i), ("rt", 2 * i + 1)], w=dkeys)

        for c in range(4):
            sa, sb = 2 * (c % 2), 2 * (c % 2) + 1
            load_w(8 + c, sa)
            load_w(12 + c, sb)
            for g in range(4):
                proj_pair(sa, sb, 128 + 512 * g, 512, qT[:, c, 512 * g:512 * (g + 1)], [("qT", 4 * g + i) for i in range(4)])
        for hk in range(2):
            sa, sb = 2 * (hk % 2), 2 * (hk % 2) + 1
            load_w(16 + hk, sa)
            load_w(18 + hk, sb)
            for g in range(5):
                n = 512 if g < 4 else 256
                proj_pair(sa, sb, 512 * g, n, kTd[:, hk, 512 * g:512 * g + n], [("kTd", 4 * g + i) for i in range(n // 128)])
            proj_pair(sa, sb, 2304, 256, kcT[:, hk, :], [("kcT", hk)], do_rope=False)
        for c_ in range(4):
            tap("qT%d" % c_, qT[:, c_, :], [("qT", i) for i in range(16)])
        for c_ in range(2):
            tap("kTd%d" % c_, kTd[:, c_, :], [("kTd", i) for i in range(18)])
            tap("kcT%d" % c_, kcT[:, c_, :], [("kcT", c_)])
        tap("vaug", vaug_f, [("vaug", i) for i in range(20)])
        if stop == "C1":
            finish()
            return nc

        R_C.reset()
        attn_all = R_C.get(16 * 256, name="attn")
        attn = v3(attn_all.bitcast(BF16), 16)
        keys("attn", 16)
        den = R_misc.get(8)
        R_B.reset()
        keys("qz", 4)
        qz_all = R_B.get(4 * 512, BF16, name="qz")
        qz = [[v3(qz_all[:, (2 * b_ + h_) * 512:(2 * b_ + h_ + 1) * 512], 4) for h_ in range(2)] for b_ in range(2)]
        mset("gpsimd", qz_all, 0.0, w=keys("qz", 4))
        scnt = [0]
        for n in range(NT):
            t = n + 1
            qb = n % 2
            cp("gpsimd", qz[qb][0][0:64, :, :], qT[0:64, :, n * 128:(n + 1) * 128], r=[("qT", n)], w=[("qz", 2 * qb)])
            cp("gpsimd", qz[qb][1][64:128, :, :], qT[64:128, :, n * 128:(n + 1) * 128], r=[("qT", n)], w=[("qz", 2 * qb + 1)])
            for hk in range(2):
                ktiles = [("k", t - 1, 2 if n == 0 else 0), ("k", t, None), ("k", t + 1, 3 if n == NT - 1 else 1),
                          ("c", 0, None), ("c", 1, None)]
                obank = 6 + (2 * n + hk) % 2
                pts = []
                for kind, idx, mtype in ktiles:
                    sb_ = scnt[0] % 6
                    ps_ = scnt[0] % 5
                    scnt[0] += 1
                    if mtype is not None:
                        mm(PB(sb_), identb, amask[:, mtype * 512:(mtype + 1) * 512], True, False,
                           r=["identb", "amask"], w=[("ps", sb_)])
                    for i in range(4):
                        h = 4 * hk + i
                        ch, half = h // 2, h % 2
                        if kind == "k":
                            lhs = kTd[:, hk, idx * 128:(idx + 1) * 128]
                            rk = [("kTd", idx)]
                        else:
                            lhs = kcT[:, hk, idx * 128:(idx + 1) * 128]
                            rk = [("kcT", hk)]
                        mm(PB(sb_)[:, i * 128:(i + 1) * 128], lhs, qz[qb][half][:, ch, :],
                           mtype is None, True, r=rk + [("qz", 2 * qb + half)], w=[("ps", sb_)])
                    act(PT[ps_], PB(sb_), AF.Exp, r=[("ps", sb_)], w=[("PT", ps_)], scale=0.125)
                    pts.append((ps_, kind, idx))
                ob = PB(obank)[:, 0:260].rearrange("p (a b) -> p a b", a=4)
                for i in range(4):
                    for j, (ps_, kind, idx) in enumerate(pts):
                        vt = idx if kind == "k" else 18 + idx
                        mm(ob[:, i, :], PT[ps_][:, i * 128:(i + 1) * 128], vaug[:, vt, hk, 0:65], j == 0, j == 4,
                           r=[("PT", ps_), ("vaug", vt)], w=[("ps", obank)])
                dn = den[:, 4 * hk:4 * hk + 4]
                tt("vector", dn, ob[:, :, 64], esink[:, 4 * hk:4 * hk + 4], ALU.add, r=[("ps", obank), "esink"], w=[("den", hk)])
                recip(dn, dn, r=[("den", hk)], w=[("den", hk)])
                tt("vector", attn[:, n, 256 * hk:256 * (hk + 1)].rearrange("p (a b) -> p a b", a=4), ob[:, :, 0:64],
                   bc(dn.unsqueeze(2), [128, 4, 64]), ALU.mult, r=[("ps", obank), ("den", hk)], w=[("attn", n)])
        tap("attn", attn_all.bitcast(BF16), [("attn", i) for i in range(16)])
        if stop == "C":
            finish()
            return nc

        R_A.reset()
        keys("wsf", 2)
        wsf_all = R_A.get(2048, name="wsf")
        wsf = [wsf_all[:, 0:1024], wsf_all[:, 1024:2048]]
        keys("wsb", 4)
        wsb_all = R_A.get(4096, BF16, name="wsb")
        wsb = [wsb_all[:, i * 1024:(i + 1) * 1024] for i in range(4)]
        keys("cacc", 2)
        cacc = [R_A.get(512, name="cacc") for _ in range(2)]
        keys("xsTr", 2)
        xsTr = [R_A.get(2048, BF16, name="xsTr") for _ in range(2)]
        xsTc = R_A.get(256, BF16, name="xsTc")
        R_m1b = Region(138, 8)
        keys("wzb", 4)
        wzb = v3(R_m1b.get(8 * 512, BF16, name="wzb"), 8)
        R_B.reset()
        keys("siluz", 16)
        siluz_all = R_B.get(16 * 256, name="siluz")
        siluz = v3(siluz_all.bitcast(BF16), 16)
        R_BC.reset()
        keys("BT", 2)
        keys("BTc", 2)
        keys("xs_tm_c", 1)
        keys("B_tm_c", 1)
        keys("CT", 2)
        BT = v3(R_BC.get(2 * 2048, BF16, name="BT"), 2)
        CT = v3(R_BC.get(2 * 2048, BF16, name="CT"), 2)
        BTc = v3(R_BC.get(2 * 256, BF16, name="BTc"), 2)
        R_tm.reset()
        keys("xs_tm", 16)
        keys("B_tm", 16)
        xs_tm = v3(R_tm.get(16 * 512, BF16, name="xs_tm"), 16)
        B_tm = v3(R_tm.get(16 * 256, BF16, name="B_tm"), 16)
        xs_tm_c = v3(R_tm.get(2 * 512, BF16, name="xs_tm_c"), 2)
        B_tm_c = v3(R_tm.get(2 * 256, BF16, name="B_tm_c"), 2)

        for piece in range(4):
            sf = wcnt[0] % 2
            wcnt[0] += 1
            dma("sync", ("wsf", sf), wsf[sf], wz_d[:, piece * 1024:(piece + 1) * 1024], w=[("wsf", sf)])
            cp("gpsimd", wzb[:, 2 * piece:2 * piece + 2, :], v3(wsf[sf], 2), r=[("wsf", sf)], w=[("wzb", piece)])
        for n in range(NT):
            t = n + 1
            bank = 6 + n % 2
            for kc in range(8):
                mm(PB(bank), hxT[:, kc, t * 128:(t + 1) * 128], wzb[:, kc, :], kc == 0, kc == 7,
                   r=[("hxT", t), ("wzb", kc // 2)], w=[("ps", bank)])
            act(siluz[:, n, :], PB(bank), AF.Silu, r=[("ps", bank)], w=[("siluz", n)])

        ccnt = [0]

        def conv_window(j, sbw, e0, nn, Lo, dst, dk, ctxw):
            bank = 2 + ccnt[0] % 4
            sl = ccnt[0] % 2
            ccnt[0] += 1
            hk_ = [("hxT", t) for t in range(e0 // 128, (e0 + nn - 1) // 128 + 1)]
            for kc in range(8):
                mm(PB(bank)[:, 0:nn], v3(wsb[sbw], 8)[:, kc, :], hxT[:, kc, e0:e0 + nn], kc == 0, kc == 7,
                   r=hk_ + [("wsb", sbw)], w=[("ps", bank)])
            wk = lambda k: convw[:, j * 5 + k:j * 5 + k + 1]
            rr = [("ps", bank), ("cacc", sl), "convw", "convb"]
            if not ctxw:
                ts("vector", cacc[sl][:, 0:Lo], PB(bank)[:, 0:Lo], wk(0), convb[:, j:j + 1], ALU.mult, ALU.add, r=rr, w=[("cacc", sl)])
                for k in range(1, 5):
                    stt(cacc[sl][:, 0:Lo], PB(bank)[:, k:k + Lo], wk(k), cacc[sl][:, 0:Lo], ALU.mult, ALU.add, r=rr, w=[("cacc", sl)])
            else:
                ts("vector", cacc[sl][:, 0:256], PB(bank)[:, 0:256], wk(2), convb[:, j:j + 1], ALU.mult, ALU.add, r=rr, w=[("cacc", sl)])
                for k, (olo, ohi, ilo) in ((0, (2, 256, 0)), (1, (1, 256, 0)), (3, (0, 255, 1)), (4, (0, 254, 2))):
                    stt(cacc[sl][:, olo:ohi], PB(bank)[:, ilo:ilo + (ohi - olo)], wk(k), cacc[sl][:, olo:ohi], ALU.mult, ALU.add, r=rr, w=[("cacc", sl)])
            act(dst, cacc[sl][:, 0:Lo], AF.Silu, r=[("cacc", sl)], w=dk)

        for j in range(8):
            sbw = j % 4
            load_w(j, sbw)
            if j < 4:
                dfull, dk = xsTr[j % 2], [("xsTr", j % 2)]
            elif j < 6:
                dfull, dk = BT[:, j - 4, :], [("BT", j - 4)]
            else:
                dfull, dk = CT[:, j - 6, :], [("CT", j - 6)]
            for w_ in range(5):
                e0 = 126 + 508 * w_
                nn, Lo = (512, 508) if w_ < 4 else (20, 16)
                conv_window(j, sbw, e0, nn, Lo, dfull[:, 508 * w_:508 * w_ + Lo], dk, False)
            if j < 6:
                dc, dck = (xsTc, ["xsTc"]) if j < 4 else (BTc[:, j - 4, :], [("BTc", j - 4)])
                conv_window(j, sbw, 2304, 256, 256, dc, dck, True)
                if j < 4:
                    src, sk_, dst_t, dst_c, col0, dname = dfull, dk, xs_tm, xs_tm_c, j * 128, "xs_tm"
                else:
                    src, sk_, dst_t, dst_c, col0, dname = dfull, dk, B_tm, B_tm_c, (j - 4) * 128, "B_tm"
                for g8 in range(2):
                    bank = g8
                    for i in range(8):
                        tile = 8 * g8 + i
                        tr(PBh(bank)[:, i * 128:(i + 1) * 128], src[:, tile * 128:(tile + 1) * 128], identb,
                           r=sk_ + ["identb"], w=[("ps", bank)])
                    cp("scalar", dst_t[:, 8 * g8:8 * g8 + 8, col0:col0 + 128], v3(PBh(bank)[:, 0:1024], 8),
                       r=[("ps", bank)], w=[(dname, 8 * g8 + i) for i in range(8)])
                bank = 0
                for i in range(2):
                    tr(PBh(bank)[:, i * 128:(i + 1) * 128], dc[:, i * 128:(i + 1) * 128], identb, r=dck + ["identb"], w=[("ps", bank)])
                cp("scalar", dst_c[:, 0:2, col0:col0 + 128], v3(PBh(bank)[:, 0:256], 2), r=[("ps", bank)], w=[(dname + "_c", 0)])

        dts_f = R_misc.get(320)
        dA_f = R_misc.get(320)
        dts, dA = v3(dts_f, 20), v3(dA_f, 20)
        tt("vector", dts, dtraw, bc(dtb.unsqueeze(1), [128, 20, 16]), ALU.add, r=keys("dtraw", 20) + ["dtb"], w=["dts"])
        act(dts_f, dts_f, AF.Exp, r=["dts"], w=["dts"])
        act(dts_f, dts_f, AF.Ln, r=["dts"], w=["dts"], bias=1.0)
        tt("vector", dA, dts, bc(aneg.unsqueeze(1), [128, 20, 16]), ALU.mult, r=["dts", "aneg"], w=["dA"])
        tap("xs_tm", xs_tm.rearrange("p a b -> p (a b)"), keys("xs_tm", 16))
        tap("B_tm", B_tm.rearrange("p a b -> p (a b)"), keys("B_tm", 16))
        tap("CT", CT.rearrange("p a b -> p (a b)"), keys("CT", 2))
        tap("xs_tm_c", xs_tm_c.rearrange("p a b -> p (a b)"), [("xs_tm_c", 0)])
        tap("B_tm_c", B_tm_c.rearrange("p a b -> p (a b)"), [("B_tm_c", 0)])
        tap("dts", dts_f, ["dts"])
        tap("siluz", siluz_all.bitcast(BF16), keys("siluz", 16))
        if stop == "D":
            finish()
            return nc

        for i_ in range(NE * CAP // 128):
            dma("sync", "G:zf", Xbuf[i_ * 128:(i_ + 1) * 128, :], zt, r=["zt"], w=["Xzero"])
        R_A.reset()
        keys("yacc", 16)
        yacc_all = R_A.get(16 * 512, name="yacc")
        yacc = v3(yacc_all, 16)
        R_hx.reset()
        ST = R_hx.get(1040, name="ST")
        hTs = [ST[:, 0:512], ST[:, 512:1024]]
        cums = [ST[:, 1024:1032], ST[:, 1032:1040]]
        hC_all = R_hx.get(1024, name="hC")
        hCs = [hC_all[:, 0:512], hC_all[:, 512:1024]]
        hTb_all = R_hx.get(1024, BF16, name="hTb")
        hTbs = [hTb_all[:, 0:512], hTb_all[:, 512:1024]]
        cumC = R_hx.get(16, name="cumC")
        e_base = R_hx.off
        ALLKEYS["ST"] = ["hT0", "hT1", "cum0", "cum1"]
        ALLKEYS["hC"] = ["hC0", "hC1"]
        ALLKEYS["hTb"] = ["hTb0", "hTb1"]
        ALLKEYS["hin"] = ["hin0", "hin1"]
        for nm_ in ("ST", "hC", "hTb", "hin"):
            for k_ in ALLKEYS[nm_]:
                S.keyname[k_] = nm_
        for nm_ in ("TdA", "ES", "cbm", "MT", "dtx", "dtxw", "xsD", "tmpy", "sm", "smx"):
            keys(nm_, 2)
        TdA = [R_hx.get(1024, name="TdA") for _ in range(2)]
        ES = [R_hx.get(1024, BF16, name="ES") for _ in range(2)]
        cbm = [R_hx.get(256, BF16, name="cbm") for _ in range(2)]
        MT = [R_hx.get(1024, BF16, name="MT") for _ in range(2)]
        dtx = [R_hx.get(512, BF16, name="dtx") for _ in range(2)]
        dtxw = [R_hx.get(512, BF16, name="dtxw") for _ in range(2)]
        xsD = [R_hx.get(512, BF16, name="xsD") for _ in range(2)]
        tmpy = [R_hx.get(512, name="tmpy") for _ in range(2)]
        sm = [R_hx.get(16, name="sm") for _ in range(2)]
        smx_ = [R_hx.get(48, name="smx") for _ in range(2)]
        Epass_f = R_misc.get(16 * 16)
        Epass = Epass_f.rearrange("p (n d h) -> p n d h", n=16, d=2)
        mset("vector", ST, 0.0, w=["hT0", "hT1", "cum0", "cum1"])
        mset("vector", hC_all, 0.0, w=["hC0", "hC1"])
        mset("vector", cumC, 0.0, w=["cumC"])
        mset("gpsimd", hTb_all, 0.0, w=["hTb0", "hTb1"])
        ecnt = [0]

        def ssd_chunk(t, n, d, xs_src, xs_k, B_src, B_k, cols, hT, hk, hTb_d, hbk, cum_d, cumk, need_y):
            sl = ecnt[0] % 2
            Y_, YO_, ST_ = 3, 4, 5
            ecnt[0] += 1
            Tri, TriK = (LE, "LE") if d == 0 else (GE, "GE")
            Str, StrK = (GT, "GT") if d == 0 else (LT, "LT")
            dAc = dA[:, t, d * 8:(d + 1) * 8]
            dtc = dts[:, t, d * 8:(d + 1) * 8]
            wj, dtw, eat, eacs, ep = [smx_[sl][:, 8 * i:8 * i + 8] for i in range(5)]
            xs3 = xs_src.rearrange("p (h c) -> p h c", h=8)
            mm(PB(2)[:, 256:264], Tri, dAc, True, True, r=[TriK, "dA"], w=[("ps", 2)])
            mm(PB(2)[:, 264:272], onesf, dAc, True, True, r=["onesf", "dA"], w=[("ps", 2)])
            cp("vector", sm[sl], PB(2)[:, 256:272], r=[("ps", 2)], w=[("sm", sl)])
            acs, atot = sm[sl][:, 0:8], sm[sl][:, 8:16]
            sx = [("smx", sl)]
            tt("vector", wj, atot, acs, ALU.subtract, r=[("sm", sl)], w=sx)
            act(wj, wj, AF.Exp, r=sx, w=sx)
            tt("vector", dtw, wj, dtc, ALU.mult, r=sx + ["dts"], w=sx)
            act(eat, atot, AF.Exp, r=[("sm", sl)], w=sx)
            if need_y:
                act(eacs, acs, AF.Exp, r=[("sm", sl)], w=sx)
                tt("vector", ep, acs, cum_d, ALU.add, r=[("sm", sl), cumk], w=sx)
                act(Epass[:, n, d, :], ep, AF.Exp, r=sx, w=[("Ep", n, d)])
            tt("vector", cum_d, cum_d, atot, ALU.add, r=[cumk, ("sm", sl)], w=[cumk])
            if need_y:
                TdA3 = v3(TdA[sl], 8)
                tt("vector", TdA3, bc(Tri.unsqueeze(1), [128, 8, 128]), bc(dAc.unsqueeze(2), [128, 8, 128]), ALU.mult,
                   r=[TriK, "dA"], w=[("TdA", sl)])
                sb0 = 0 if sl == 0 else 6
                for hh in range(2):
                    mm(PB(sb0 + hh), Str, TdA[sl][:, hh * 512:(hh + 1) * 512], True, True, r=[StrK, ("TdA", sl)], w=[("ps", sb0 + hh)])
                for hh in range(2):
                    act(ES[sl][:, hh * 512:(hh + 1) * 512], PB(sb0 + hh), AF.Exp, r=[("ps", sb0 + hh)], w=[("ES", sl)])
                for g in range(2):
                    mm(PB(2)[:, g * 128:(g + 1) * 128], BT[:, g, cols], CT[:, g, cols], True, True,
                       r=[("BT", g), ("CT", g)], w=[("ps", 2)])
                cbm3 = v3(cbm[sl], 2)
                tt("vector", cbm3, v3(PB(2)[:, 0:256], 2), bc(Tri.unsqueeze(1), [128, 2, 128]), ALU.mult,
                   r=[("ps", 2), TriK], w=[("cbm", sl)])
                MT4 = MT[sl].rearrange("p (g r i) -> p g r i", g=2, r=4)
                ES4 = ES[sl].rearrange("p (g r i) -> p g r i", g=2, r=4)
                tt("vector", MT4, ES4, bc(cbm3.unsqueeze(2), [128, 2, 4, 128]), ALU.mult,
                   r=[("ES", sl), ("cbm", sl)], w=[("MT", sl)])
                dtx3 = v3(dtx[sl], 8)
                tt("vector", dtx3, xs3, bc(dtc.unsqueeze(2), [128, 8, 64]), ALU.mult, r=[xs_k, "dts"], w=[("dtx", sl)])
                if d == 0:
                    tt("gpsimd", xsD[sl], xs_src, D512, ALU.mult, r=[xs_k, "D512"], w=[("xsD", sl)])
                    mm(PB(Y_), identb, xsD[sl], True, False, r=["identb", ("xsD", sl)], w=[("ps", Y_)])
                MT3 = v3(MT[sl], 8)
                for h in range(8):
                    mm(PB(Y_)[:, h * 64:(h + 1) * 64], MT3[:, h, :], dtx3[:, h, :], d != 0, True,
                       r=[("MT", sl), ("dtx", sl)], w=[("ps", Y_)])
                for g in range(2):
                    mm(PB(YO_)[:, g * 256:(g + 1) * 256], CT[:, g, cols], hTb_d[:, g * 256:(g + 1) * 256], True, True,
                       r=[("CT", g), hbk], w=[("ps", YO_)])
                tt("vector", v3(tmpy[sl], 8), v3(PB(YO_), 8), bc(eacs.unsqueeze(2), [128, 8, 64]), ALU.mult,
                   r=[("ps", YO_)] + sx, w=[("tmpy", sl)])
                if d == 0:
                    tt("vector", yacc[:, n, :], tmpy[sl], PB(Y_), ALU.add, r=[("tmpy", sl), ("ps", Y_)], w=[("yacc", n)])
                else:
                    tt("gpsimd", yacc[:, n, :], yacc[:, n, :], tmpy[sl], ALU.add, r=[("tmpy", sl), ("yacc", n)], w=[("yacc", n)])
                    tt("vector", yacc[:, n, :], yacc[:, n, :], PB(Y_), ALU.add, r=[("ps", Y_), ("yacc", n)], w=[("yacc", n)])
            dtxw3 = v3(dtxw[sl], 8)
            tt("vector", dtxw3, xs3, bc(dtw.unsqueeze(2), [128, 8, 64]), ALU.mult, r=[xs_k] + sx, w=[("dtxw", sl)])
            for g in range(2):
                mm(PB(ST_)[:, g * 256:(g + 1) * 256], B_src[:, g * 128:(g + 1) * 128], dtxw[sl][:, g * 256:(g + 1) * 256],
                   True, True, r=[B_k, ("dtxw", sl)], w=[("ps", ST_)])
            tt("vector", v3(hT, 8), v3(hT, 8), bc(eat.unsqueeze(2), [128, 8, 64]), ALU.mult, r=[hk] + sx, w=[hk])
            tt("vector", hT, hT, PB(ST_), ALU.add, r=[hk, ("ps", ST_)], w=[hk])
            if hTb_d is not None:
                cp("scalar", hTb_d, hT, r=[hk], w=[hbk])

        for ci in (0, 1):
            ssd_chunk(18 + ci, None, 0, xs_tm_c[:, ci, :], ("xs_tm_c", 0), B_tm_c[:, ci, :], ("B_tm_c", 0), None,
                      hCs[0], "hC0", None, None, cumC[:, 0:8], "cumC", False)
        for ci in (1, 0):
            ssd_chunk(18 + ci, None, 1, xs_tm_c[:, ci, :], ("xs_tm_c", 0), B_tm_c[:, ci, :], ("B_tm_c", 0), None,
                      hCs[1], "hC1", None, None, cumC[:, 8:16], "cumC", False)
        for n in range(NT):
            ssd_chunk(n + 1, n, 0, xs_tm[:, n, :], ("xs_tm", n), B_tm[:, n, :], ("B_tm", n), slice(n * 128, (n + 1) * 128),
                      hTs[0], "hT0", hTbs[0], "hTb0", cums[0], "cum0", True)
        for n in reversed(range(NT)):
            ssd_chunk(n + 1, n, 1, xs_tm[:, n, :], ("xs_tm", n), B_tm[:, n, :], ("B_tm", n), slice(n * 128, (n + 1) * 128),
                      hTs[1], "hT1", hTbs[1], "hTb1", cums[1], "cum1", True)
        tap("ST", ST, ["hT0", "hT1", "cum0", "cum1"])
        tap("hC", hC_all, ["hC0", "hC1"])

        dma("gpsimd", "xin", xin[:, :], ST, r=["hT0", "hT1", "cum0", "cum1"], w=["xin"])
        S.op("gpsimd", lambda e: e.collective_compute("AllGather", ALU.bypass, replica_groups=[[0, 1, 2, 3], [4, 5, 6, 7]],
                                                      ins=[xin.ap().opt()], outs=[xout.ap().opt()]),
             ["xin"], ["xout"], dsem="C:cc")
        R_hx2 = Region(86 + e_base / 256.0, 40 - e_base / 256.0)
        G_f = R_hx2.get(4 * 1040, name="G")
        G3 = v3(G_f, 4)
        hin = R_hx2.get(1024, name="hin")
        hinb = R_hx2.get(1024, BF16, name="hinb")
        Dall = R_hx2.get(64, name="Dall")
        al = R_hx2.get(8, name="al")
        keys("tmpy2", 2)
        tmpy2 = [R_hx2.get(512, name="tmpy2") for _ in range(2)]
        dma("gpsimd", "G", G3, xout.ap().rearrange("(r p) f -> p r f", p=128), r=["xout"], w=["G"])
        Dall3 = v3(Dall, 4)
        act(Dall3, G3[:, :, 1024:1040], AF.Exp, r=["G"], w=["Dall"])
        for d in range(2):
            hd = hin[:, d * 512:(d + 1) * 512]
            hk = "hin%d" % d
            cp("vector", hd, hCs[d], r=["hC%d" % d], w=[hk])
            order = range(4) if d == 0 else reversed(range(4))
            for i in order:
                mcol = mv[:, 4 * d + i:4 * d + i + 1]
                ts("vector", al, Dall3[:, i, 8 * d:8 * d + 8], -1.0, mcol, ALU.add, ALU.mult, r=["Dall", "mv"], w=["al"])
                ts("vector", al, al, 1.0, None, ALU.add, r=["al"], w=["al"])
                tt("vector", v3(hd, 8), v3(hd, 8), bc(al.unsqueeze(2), [128, 8, 64]), ALU.mult, r=[hk, "al"], w=[hk])
                stt(hd, G3[:, i, d * 512:(d + 1) * 512], mcol, hd, ALU.mult, ALU.add, r=["G", "mv", hk], w=[hk])
        cp("scalar", hinb, hin, r=["hin0", "hin1"], w=["hinb"])
        tap("hin", hin, ["hin0", "hin1"])
        bcnt = [0]
        for n in range(NT):
            for d in range(2):
                bank = bcnt[0] % 4
                sl = bcnt[0] % 2
                bcnt[0] += 1
                for g in range(2):
                    mm(PB(bank)[:, g * 256:(g + 1) * 256], CT[:, g, n * 128:(n + 1) * 128],
                       hinb[:, d * 512 + g * 256:d * 512 + (g + 1) * 256], True, True, r=[("CT", g), "hinb"], w=[("ps", bank)])
                tt("vector", v3(tmpy2[sl], 8), v3(PB(bank), 8), bc(Epass[:, n, d, :].unsqueeze(2), [128, 8, 64]), ALU.mult,
                   r=[("ps", bank), ("Ep", n, d)], w=[("tmpy2", sl)])
                tt("gpsimd", yacc[:, n, :], yacc[:, n, :], tmpy2[sl], ALU.add, r=[("tmpy2", sl), ("yacc", n)], w=[("yacc", n)])
        tap("yacc", yacc_all, keys("yacc", 16))
        tap("CTe", CT.rearrange("p a b -> p (a b)"), keys("CT", 2))
        tap("BTe", BT.rearrange("p a b -> p (a b)"), keys("BT", 2))
        if stop == "E":
            finish()
            return nc

        R_x2 = Region(154, 44)
        keys("woutb", 8)
        woutb = v3(R_x2.get(8 * 1024, BF16, name="woutb"), 8)
        keys("wof", 2)
        wof = [R_x2.get(1024, name="wof") for _ in range(2)]
        for nm_ in ("yg", "mixb", "mixT", "xres", "tmpF"):
            keys(nm_, 2)
        yg = [R_x2.get(512, name="yg") for _ in range(2)]
        mixb = [R_x2.get(512, BF16, name="mixb") for _ in range(2)]
        mixT = [R_x2.get(1024, BF16, name="mixT") for _ in range(2)]
        xres = [R_x2.get(1024, name="xres") for _ in range(2)]
        R_hx3 = Region(86, 40)
        keys("h2b", 16)
        h2b = v3(R_hx3.get(16 * 1024, BF16, name="h2b"), 16)
        h2f = R_hx3.get(1024, name="h2f")
        h2Tf = R_hx3.get(1024, name="h2Tf")
        R_m1c = Region(138, 8)
        keys("lg", 16)
        lg_f = R_m1c.get(16 * 36, name="lg")
        lg = v3(lg_f, 16)
        tmpF = [R_m1c.get(512, name="tmpF") for _ in range(2)]
        st_f = R_misc.get(64)
        st = v3(st_f, 16)
        x1A = yacc
        x1B = v3(siluz_all, 16)
        x1C = v3(attn_all, 16)
        w_out_v = w_out.rearrange("(kc p) n -> p kc n", p=128)
        for kc in range(8):
            sf = kc % 2
            dma("sync", ("wof", sf), wof[sf], w_out_v[:, kc, :], w=[("wof", sf)])
            cp("gpsimd", woutb[:, kc, :], wof[sf], r=[("wof", sf)], w=[("woutb", kc)])
        wr3 = v3(wr, 8)
        g1k = [("g1", 0), ("g1", 1)]
        for n in range(NT):
            sl = n % 2
            tt("vector", yg[sl], yacc[:, n, :], siluz[:, n, :], ALU.mult, r=[("yacc", n), ("siluz", n)], w=[("yg", sl)])
            act(mixb[sl], yg[sl], AF.Square, r=[("yg", sl)], w=[("mixb", sl), ("st", n)], accum=st[:, n, 0:1])
            act(st[:, n, 0:1], st[:, n, 0:1], AF.Sqrt, r=[("st", n)], w=[("st", n)], bias=EPS, scale=1.0 / 512)
            recip(st[:, n, 0:1], st[:, n, 0:1], r=[("st", n)], w=[("st", n)])
            stt(mixb[sl], yg[sl], st[:, n, 0:1], ssdn, ALU.mult, ALU.mult, r=[("yg", sl), ("st", n), "ssdn"], w=[("mixb", sl)])
            bank = n % 2
            for kc in range(4):
                tr(PBh(bank)[:, kc * 128:(kc + 1) * 128], mixb[sl][:, kc * 128:(kc + 1) * 128], identb,
                   r=[("mixb", sl), "identb"], w=[("ps", bank)])
            for kc in range(4):
                tr(PBh(bank)[:, (4 + kc) * 128:(5 + kc) * 128], attn[:, n, kc * 128:(kc + 1) * 128], identb,
                   r=[("attn", n), "identb"], w=[("ps", bank)])
            cp("scalar", v3(mixT[sl], 8), v3(PBh(bank)[:, 0:1024], 8), r=[("ps", bank)], w=[("mixT", sl)])
            dma("sync", ("xres", sl), xres[sl], xe[(n + 1) * 128:(n + 2) * 128, :], w=[("xres", sl)])
            mixT3 = v3(mixT[sl], 8)
            for half in range(2):
                b2 = 2 + 2 * (n % 2) + half
                for kc in range(8):
                    mm(PB(b2), mixT3[:, kc, :], woutb[:, kc, half * 512:(half + 1) * 512], kc == 0, kc == 7,
                       r=[("mixT", sl), ("woutb", kc)], w=[("ps", b2)])
                tt("vector", tmpF[half], PB(b2), g1[:, half * 512:(half + 1) * 512], ALU.mult,
                   r=[("ps", b2)] + g1k, w=[("tmpF", half)])
                if half == 0:
                    tt("gpsimd", x1A[:, n, :], tmpF[0], xres[sl][:, 0:512], ALU.add, r=[("tmpF", 0), ("xres", sl)], w=[("yacc", n)])
                else:
                    tt("gpsimd", x1B[:, n, :], tmpF[1][:, 0:256], xres[sl][:, 512:768], ALU.add,
                       r=[("tmpF", 1), ("xres", sl)], w=[("siluz", n)])
                    tt("gpsimd", x1C[:, n, :], tmpF[1][:, 256:512], xres[sl][:, 768:1024], ALU.add,
                       r=[("tmpF", 1), ("xres", sl)], w=[("attn", n)])
            pieces = ((x1A[:, n, :], ("yacc", n), 0, 512), (x1B[:, n, :], ("siluz", n), 512, 768), (x1C[:, n, :], ("attn", n), 768, 1024))
            for pi, (xp, xk, c0, c1) in enumerate(pieces):
                act(h2f[:, c0:c1], xp, AF.Square, r=[xk], w=["h2f", ("st", n)], accum=st[:, n, 1 + pi:2 + pi])
            tt("vector", st[:, n, 1:2], st[:, n, 1:2], st[:, n, 2:3], ALU.add, r=[("st", n)], w=[("st", n)])
            tt("vector", st[:, n, 1:2], st[:, n, 1:2], st[:, n, 3:4], ALU.add, r=[("st", n)], w=[("st", n)])
            act(st[:, n, 1:2], st[:, n, 1:2], AF.Sqrt, r=[("st", n)], w=[("st", n)], bias=EPS, scale=1.0 / D)
            recip(st[:, n, 1:2], st[:, n, 1:2], r=[("st", n)], w=[("st", n)])
            for xp, xk, c0, c1 in pieces:
                stt(h2f[:, c0:c1], xp, st[:, n, 1:2], gsc2[:, c0:c1], ALU.mult, ALU.mult,
                    r=[xk, ("st", n), ("gsc2", 0), ("gsc2", 1)], w=["h2f"])
            tt("gpsimd", h2f, h2f, sh2, ALU.add, r=["h2f", ("sh2", 0), ("sh2", 1)], w=["h2f"])
            cp("scalar", h2b[:, n, :], h2f, r=["h2f"], w=[("h2b", n)])
            for kc in range(8):
                tr(PB(6 + kc // 4)[:, (kc % 4) * 128:(kc % 4 + 1) * 128], h2f[:, kc * 128:(kc + 1) * 128], identf,
                   r=["h2f", "identf"], w=[("ps", 6 + kc // 4)])
            h2Tf3 = v3(h2Tf, 8)
            for hb in range(2):
                cp("vector", h2Tf3[:, 4 * hb:4 * hb + 4, :], v3(PB(6 + hb), 4), r=[("ps", 6 + hb)], w=["h2Tf"])
            rb = 2 + 2 * (n % 2)
            for kc in range(8):
                mm(PB(rb)[:, 0:36], h2Tf3[:, kc, :], wr3[:, kc, :], kc == 0, kc == 7, r=["h2Tf", "wr"], w=[("ps", rb)])
            tt("vector", lg[:, n, :], PB(rb)[:, 0:36], brb, ALU.add, r=[("ps", rb), "brb"], w=[("lg", n)])
        tap("x1a", yacc_all, keys("yacc", 16))
        tap("x1b", siluz_all, keys("siluz", 16))
        tap("x1c", attn_all, keys("attn", 16))
        tap("lg", lg_f, keys("lg", 16))

        R_x3 = Region(154, 44)
        S.keyname["tk"] = "tkall"
        S.alias["tkall"] = [k_ for nm_ in ("BT", "CT", "BTc", "xs_tm", "B_tm", "xs_tm_c", "B_tm_c", "woutb", "wof", "yg", "mixb", "mixT", "xres") for k_ in ALLKEYS.get(nm_, [nm_])]
        tk = lambda nelem, nm: R_x3.get(nelem, name=nm)
        gmax, gsum, gw, m1, m2, w2, dn2, c1, c2 = [tk(16, "tk%d" % i) for i in range(9)]
        gsel, gexp, gselm = [v3(tk(64, "tk1%d" % i), 16) for i in range(3)]
        lm, sel1, lm2, sel2 = [v3(tk(512, "tk2%d" % i), 16) for i in range(4)]
        lgG, lgE = lg[:, :, 0:4], lg[:, :, 4:36]
        KL = keys("lg", 16)
        BIG = 30000.0
        b4 = lambda a: bc(a.unsqueeze(2), [128, 16, 4])
        b32 = lambda a: bc(a.unsqueeze(2), [128, 16, 32])
        red(gmax, lgG, ALU.max, r=KL, w=["tk"])
        tt("vector", gsel, lgG, b4(gmax), ALU.is_equal, r=KL + ["tk"], w=["tk"])
        tt("vector", gexp, lgG, b4(gmax), ALU.subtract, r=KL + ["tk"], w=["tk"])
        act(gexp, gexp, AF.Exp, r=["tk"], w=["tk"])
        red(gsum, gexp, ALU.add, r=["tk"], w=["tk"])
        recip(gw, gsum, r=["tk"], w=["tk"])
        ts("vector", gselm, gsel, -1.0, BIG, ALU.add, ALU.mult, r=["tk"], w=["tk"])
        lm4 = lm.rearrange("p n (g j) -> p n g j", g=4)
        lgE4 = lgE.rearrange("p n (g j) -> p n g j", g=4)
        tt("vector", lm4, lgE4, bc(gselm.unsqueeze(3), [128, 16, 4, 8]), ALU.add, r=KL + ["tk"], w=["tk"])
        red(m1, lm, ALU.max, r=["tk"], w=["tk"])
        tt("vector", sel1, lm, b32(m1), ALU.is_equal, r=["tk"], w=["tk"])
        stt(lm2, sel1, -BIG, lm, ALU.mult, ALU.add, r=["tk"], w=["tk"])
        red(m2, lm2, ALU.max, r=["tk"], w=["tk"])
        tt("vector", sel2, lm2, b32(m2), ALU.is_equal, r=["tk"], w=["tk"])
        tt("vector", w2, m2, m1, ALU.subtract, r=["tk"], w=["tk"])
        act(w2, w2, AF.Exp, r=["tk"], w=["tk"])
        ts("vector", dn2, w2, 1.0, None, ALU.add, r=["tk"], w=["tk"])
        recip(dn2, dn2, r=["tk"], w=["tk"])
        tt("vector", c1, gw, dn2, ALU.mult, r=["tk"], w=["tk"])
        tt("vector", c2, c1, w2, ALU.mult, r=["tk"], w=["tk"])
        Mm_f = tk(512, "tk30")
        Mm = v3(Mm_f, 16)
        tt("vector", Mm, sel1, sel2, ALU.add, r=["tk"], w=["tk"])
        for i in range(9):
            ALLKEYS["tk%d" % i] = ["tk"]
        for i in range(3):
            ALLKEYS["tk1%d" % i] = ["tk"]
        for i in range(4):
            ALLKEYS["tk2%d" % i] = ["tk"]
        if stop == "F":
            finish()
            return nc

        I32 = mybir.dt.int32
        BIGI = float(NE * CAP)
        eCi = tk(32, "tk40").bitcast(I32)
        eC = tk(32, "tk41")
        Mcum_f = tk(512, "tk42")
        Mcum = v3(Mcum_f, 16)
        slot = v3(tk(512, "tk43"), 16)
        okm = v3(tk(512, "tk44"), 16)
        tmpk = v3(tk(512, "tk45"), 16)
        dfl = tk(32, "tk46")
        wsel = R_misc.get(96)
        wc = [wsel[:, 0:16], wsel[:, 16:32]]
        di = [wsel[:, 32:48].bitcast(I32), wsel[:, 48:64].bitcast(I32)]
        dg = [wsel[:, 64:80].bitcast(I32), wsel[:, 80:96].bitcast(I32)]
        for i in range(40, 47):
            ALLKEYS["tk%d" % i] = ["tk"]
        ALLKEYS["tk30"] = ["tk"]
        S.op("gpsimd", lambda e: e.iota(out=eCi, pattern=[[CAP, 32]], base=0, channel_multiplier=0), (), ["eCi"])
        cp("vector", eC, eCi, r=["eCi"], w=["tk"])
        mset("vector", Mcum[:, 0, :], 0.0, w=["tk"])
        for n in range(1, NT):
            tt("vector", Mcum[:, n, :], Mcum[:, n - 1, :], Mm[:, n - 1, :], ALU.add, r=["tk"], w=["tk"])
        mm(PB(0), LT, Mm_f, True, False, r=["LT", "tk"], w=[("ps", 0)])
        mm(PB(0), onesf, Mcum_f, False, True, r=["onesf", "tk"], w=[("ps", 0)])
        ts("vector", okm, v3(PB(0), 16), float(CAP), None, ALU.is_lt, r=[("ps", 0)], w=["tk"])
        tt("vector", slot, v3(PB(0), 16), bc(eC.unsqueeze(1), [128, 16, 32]), ALU.add, r=[("ps", 0), "tk"], w=["tk"])
        ts("vector", slot, slot, -BIGI, None, ALU.add, r=["tk"], w=["tk"])
        tt("vector", slot, slot, okm, ALU.mult, r=["tk"], w=["tk"])
        ts("vector", slot, slot, BIGI, None, ALU.add, r=["tk"], w=["tk"])
        for k, selk in ((0, sel1), (1, sel2)):
            tt("vector", tmpk, selk, slot, ALU.mult, r=["tk"], w=["tk"])
            red(dfl[:, 16 * k:16 * k + 16], tmpk, ALU.add, r=["tk"], w=["tk"])
        cp("vector", di[0], dfl[:, 0:16], r=["tk"], w=["wsel"])
        cp("vector", di[1], dfl[:, 16:32], r=["tk"], w=["wsel"])
        ts("vector", dfl, dfl, BIGI - 1.0, None, ALU.min, r=["tk"], w=["tk"])
        cp("vector", dg[0], dfl[:, 0:16], r=["tk"], w=["wsel"])
        cp("vector", dg[1], dfl[:, 16:32], r=["tk"], w=["wsel"])
        cp("vector", wc[0], c1, r=["tk"], w=["wsel"])
        cp("vector", wc[1], c2, r=["tk"], w=["wsel"])
        tap("dfl", dfl, ["tk"])
        tap("wsel", wsel, ["wsel"])
        tap("Mm", Mm_f, ["tk"])
        if stop == "G":
            finish()
            return nc
        for n in range(NT):
            for k in range(2):
                S.op("gpsimd", lambda e, n=n, k=k: e.indirect_dma_start(
                    out=Xbuf[:, :], out_offset=bass.IndirectOffsetOnAxis(ap=di[k][:, n:n + 1], axis=0),
                    in_=h2b[:, n, :], in_offset=None, bounds_check=NE * CAP - 1, oob_is_err=False),
                    ["wsel", ("h2b", n), "Xzero"], ["Xbuf"], dsem="G:scat")

        R_w1 = Region(134, 12)
        R_w2 = Region(154, 44)
        for nm_ in ("wgb", "wub", "wdb"):
            keys(nm_, 2)
        wgb = [v3(R_w2.get(8 * 512, BF16, name="wgb"), 8) for _ in range(2)]
        wub = [v3(R_w2.get(8 * 512, BF16, name="wub"), 8) for _ in range(2)]
        wdb = [v3(R_w2.get(4 * 1024, BF16, name="wdb"), 4), v3(R_w1.get(4 * 1024, BF16, name="wdb"), 4)]
        keys("sg", 2)
        sg = [R_w2.get(512, BF16, name="sg") for _ in range(2)]
        R_g = Region(86, 32)
        keys("Xg", 2)
        keys("xgT", 2)
        Xg = [v3(R_g.get(4 * 1024, BF16, name="Xg"), 4) for _ in range(2)]
        xgT = [v3(R_g.get(8 * 512, BF16, name="xgT"), 8) for _ in range(2)]
        R_hx4 = Region(86 + 32, 8)
        ybf = R_hx4.get(4 * 1024, BF16, name="yb")
        yb = v3(ybf, 4)
        R_m1d = Region(126, 8)
        keys("actT", 2)
        actT = [v3(R_m1d.get(4 * 512, BF16, name="actT"), 4) for _ in range(2)]
        ALLKEYS["h2f"] = ["h2f"]
        ALLKEYS["h2Tf"] = ["h2Tf"]
        g2k = [("g2", 0), ("g2", 1)]
        mcnt = [0]
        pcnt = [0]
        dcnt = [0]
        ceng = ["gpsimd", "vector", "gpsimd", "scalar"]
        n_exp = NE if stop is None else 2

        def load_x(e_):
            dma("sync", ("Xg", e_ % 2), Xg[e_ % 2], Xbuf[e_ * CAP:(e_ + 1) * CAP, :].rearrange("(t p) d -> p t d", p=128),
                r=["Xbuf"], w=[("Xg", e_ % 2)])

        for e in range(n_exp):
            wb = e % 2
            xs_ = e % 2
            wg_v = w_gate[e % ne_decl].rearrange("(kc p) f -> p kc f", p=128)
            wu_v = w_up[e % ne_decl].rearrange("(kc p) f -> p kc f", p=128)
            wd_v = w_down[e % ne_decl].rearrange("(fc p) n -> p fc n", p=128)
            if e == 0:
                load_x(0)
            if e + 1 < n_exp:
                load_x(e + 1)
            dma("gpsimd", ("wgb", wb), wgb[wb], wg_v, w=[("wgb", wb)])
            dma("gpsimd", ("wub", wb), wub[wb], wu_v, w=[("wub", wb)])
            dma("gpsimd", ("wdb", wb), wdb[wb], wd_v, w=[("wdb", wb)])
            for ti in range(4):
                bank = 6 + ti % 2
                for kc in range(8):
                    tr(PBh(bank)[:, kc * 128:(kc + 1) * 128], Xg[xs_][:, ti, kc * 128:(kc + 1) * 128], identb,
                       r=[("Xg", xs_), "identb"], w=[("ps", bank)])
                cp("scalar", xgT[xs_][:, :, ti * 128:(ti + 1) * 128], v3(PBh(bank)[:, 0:1024], 8), r=[("ps", bank)], w=[("xgT", xs_)])
            asl = e % 2
            for fc in range(4):
                pi = pcnt[0] % 2
                pcnt[0] += 1
                bg, bu = 2 * pi, 2 * pi + 1
                for kc in range(8):
                    mm(PB(bg), wgb[wb][:, kc, fc * 128:(fc + 1) * 128], xgT[xs_][:, kc, :], kc == 0, kc == 7,
                       r=[("xgT", xs_), ("wgb", wb)], w=[("ps", bg)])
                for kc in range(8):
                    mm(PB(bu), wub[wb][:, kc, fc * 128:(fc + 1) * 128], xgT[xs_][:, kc, :], kc == 0, kc == 7,
                       r=[("xgT", xs_), ("wub", wb)], w=[("ps", bu)])
                act(sg[pi], PB(bg), AF.Silu, r=[("ps", bg)], w=[("sg", pi)])
                tt("vector", actT[asl][:, fc, :], sg[pi], PB(bu), ALU.mult, r=[("sg", pi), ("ps", bu)], w=[("actT", asl)])
            for ti in range(4):
                for half in range(2):
                    bank = 4 + dcnt[0] % 2
                    dcnt[0] += 1
                    for fc in range(4):
                        mm(PB(bank), actT[asl][:, fc, ti * 128:(ti + 1) * 128], wdb[wb][:, fc, half * 512:(half + 1) * 512],
                           fc == 0, fc == 3, r=[("actT", asl), ("wdb", wb)], w=[("ps", bank)])
                    cp("scalar" if half == 0 else "vector", yb[:, ti, half * 512:(half + 1) * 512], PB(bank), r=[("ps", bank)], w=["yb"])
            dma("sync", "yst", Ybuf[e * CAP:(e + 1) * CAP, :].rearrange("(t p) d -> p t d", p=128), yb, r=["yb"], w=["Ybuf"])

        R_g2 = Region(86, 32)
        keys("yg", 8)
        ygb = [R_g2.get(1024, BF16, name="yg") for _ in range(8)]
        keys("tmpg", 4)
        tmpg = [R_g2.get(1024, name="tmpg") for _ in range(4)]
        for i_ in range(8):
            mset("gpsimd", ygb[i_], 0.0, w=[("yg", i_)])
        for n in range(NT):
            for k in range(2):
                gi_ = 2 * (n % 4) + k
                S.op("gpsimd", lambda e, n=n, k=k, gi_=gi_: e.indirect_dma_start(
                    out=ygb[gi_], out_offset=None, in_=Ybuf[:, :],
                    in_offset=bass.IndirectOffsetOnAxis(ap=dg[k][:, n:n + 1], axis=0)),
                    ["wsel", "Ybuf"], [("yg", gi_)], dsem=("ygd", gi_))
                wcol = wc[k][:, n:n + 1]
                tj = gi_ % 4
                tt("vector" if k == 0 else "gpsimd", tmpg[tj], ygb[gi_], g2, ALU.mult, r=[("yg", gi_)] + g2k, w=[("tmpg", tj)])
                stt(x1A[:, n, :], tmpg[tj][:, 0:512], wcol, x1A[:, n, :], ALU.mult, ALU.add,
                    r=[("tmpg", tj), "wsel", ("yacc", n)], w=[("yacc", n)])
                stt(x1B[:, n, :], tmpg[tj][:, 512:768], wcol, x1B[:, n, :], ALU.mult, ALU.add,
                    r=[("tmpg", tj), "wsel", ("siluz", n)], w=[("siluz", n)])
                stt(x1C[:, n, :], tmpg[tj][:, 768:1024], wcol, x1C[:, n, :], ALU.mult, ALU.add,
                    r=[("tmpg", tj), "wsel", ("attn", n)], w=[("attn", n)])

        R_fin = Region(134, 12)
        nfb = R_fin.get(D, name="nfb")
        keys("ot", 2)
        ot = [R_fin.get(D, name="ot") for _ in range(2)]
        st2_f = R_hx4.get(64, name="st2") if False else R_misc.get(64)
        st2 = v3(st2_f, 16)
        dma("sync", "G:nfb", nfb, normf.partition_broadcast(128), w=["nfb"])
        for n in range(NT):
            sl = n % 2
            pieces = ((x1A[:, n, :], ("yacc", n), 0, 512), (x1B[:, n, :], ("siluz", n), 512, 768), (x1C[:, n, :], ("attn", n), 768, 1024))
            for pi, (xp, xk, c0, c1_) in enumerate(pieces):
                act(ot[sl][:, c0:c1_], xp, AF.Square, r=[xk], w=[("ot", sl), ("st2", n)], accum=st2[:, n, pi:pi + 1])
            tt("vector", st2[:, n, 0:1], st2[:, n, 0:1], st2[:, n, 1:2], ALU.add, r=[("st2", n)], w=[("st2", n)])
            tt("vector", st2[:, n, 0:1], st2[:, n, 0:1], st2[:, n, 2:3], ALU.add, r=[("st2", n)], w=[("st2", n)])
            act(st2[:, n, 0:1], st2[:, n, 0:1], AF.Sqrt, r=[("st2", n)], w=[("st2", n)], bias=EPS, scale=1.0 / D)
            recip(st2[:, n, 0:1], st2[:, n, 0:1], r=[("st2", n)], w=[("st2", n)])
            for xp, xk, c0, c1_ in pieces:
                stt(ot[sl][:, c0:c1_], xp, st2[:, n, 0:1], nfb[:, c0:c1_], ALU.mult, ALU.mult,
                    r=[xk, ("st2", n), "nfb"], w=[("ot", sl)])
            dma("sync", ("ot", sl), out[n * 128:(n + 1) * 128, :], ot[sl], r=[("ot", sl)], w=[("out", n)])
        finish()
        return nc
def _rope_tables():
    pos = np.arange(8192)
    row, col = pos // 64, pos % 64
    inv = 10000.0 ** (-np.arange(16, dtype=np.float64) / 16)
    ang = np.concatenate([row[:, None] * inv, col[:, None] * inv], axis=-1)
    ang = np.concatenate([ang, ang], axis=-1)
    cos, sin = np.cos(ang), np.sin(ang)
    sgn = np.where(np.arange(64) < 32, -1.0, 1.0)
    return cos.T.astype(np.float32), (sin * sgn).T.astype(np.float32)


def _fm(w, cols):
    wc = w[:, cols]
    n = wc.shape[1]
    return np.ascontiguousarray(wc.reshape(8, 128, n).transpose(1, 0, 2)).reshape(128, 8 * n)


def prep_inputs(inputs):
    f = lambda k: np.asarray(inputs[k], np.float32)
    x, c, ctx, c_ctx = f("x"), f("c"), f("ctx"), f("c_ctx")
    w_in = f("w_in")[0]
    ar = np.arange
    chunks = [512 + 128 * j + ar(128) for j in range(8)]
    chunks += [1552 + 128 * cc + ar(128) for cc in range(4)]
    perm = (ar(64) + 32) % 64
    chunks += [1552 + 128 * cc + np.concatenate([perm, 64 + perm]) for cc in range(4)]
    chunks += [2064 + 64 * hk + np.concatenate([ar(64), ar(64)]) for hk in range(2)]
    chunks += [2064 + 64 * hk + np.concatenate([perm, perm]) for hk in range(2)]
    wfm = np.stack([_fm(w_in, cc) for cc in chunks])
    wvd = _fm(w_in, np.concatenate([2192 + ar(128), 1536 + ar(16)]))
    wz = _fm(w_in, ar(512))
    convw = np.ascontiguousarray(f("conv_w")[0].T.reshape(8, 128, 5).transpose(1, 0, 2)).reshape(128, 40)
    convb = np.ascontiguousarray(f("conv_b")[0].reshape(8, 128).T)
    wr = _fm(np.concatenate([f("w_group")[0], f("w_expert")[0]], axis=1), ar(36))
    br = np.concatenate([f("b_group")[0], f("b_expert")[0]])
    cosT, sinT = _rope_tables()
    jj, ii = np.meshgrid(ar(128), ar(128), indexing="ij")
    m_prev = np.where(jj >= ii, 0.0, NEG).astype(np.float32)
    m_next = np.where(jj <= ii, 0.0, NEG).astype(np.float32)
    m_none = np.full((128, 128), NEG, np.float32)
    shared = dict(
        wfm=wfm, wvd=wvd, wz=wz, w_ada=f("w_ada")[0], b_ada=f("b_ada")[0], norm1=f("norm1")[0],
        convw=convw, convb=convb, dtb=f("dt_bias")[0].reshape(16), alog=f("a_log")[0].reshape(16),
        dskip=f("d_skip")[0], ssdnorm=f("ssd_norm")[0], sinks=f("attn_sinks")[0], w_out=f("w_out")[0],
        norm2=f("norm2")[0], wr=wr, br=br, w_gate=f("w_gate")[0], w_up=f("w_up")[0], w_down=f("w_down")[0],
        normf=f("norm_final"))
    maps = []
    for r in range(NCORES):
        b, s = r // 4, r % 4
        t0 = s * T
        xe = np.zeros((2304, D), np.float32)
        lo, hi = max(t0 - 128, 0), min(t0 + 2176, 8192)
        xe[lo - (t0 - 128):hi - (t0 - 128)] = x[b, lo:hi]
        rope = np.zeros((128, 2 * 2304), np.float32)
        for half in range(2):
            rope[half * 64:(half + 1) * 64, lo - (t0 - 128):hi - (t0 - 128)] = cosT[:, lo:hi]
            rope[half * 64:(half + 1) * 64, 2304 + lo - (t0 - 128):2304 + hi - (t0 - 128)] = sinT[:, lo:hi]
        cfm = np.zeros((128, 16), np.float32)
        cfm[:, 0:8] = c[b].reshape(8, 128).T
        cfm[:, 8:16] = c_ctx.reshape(8, 128).T
        vmask = np.zeros((128, 4), np.float32)
        vmask[:, 0] = 1.0 if s > 0 else 0.0
        vmask[:, 1] = 1.0 if s < 3 else 0.0
        mvec = np.zeros((128, 8), np.float32)
        for i in range(4):
            mvec[:, i] = 1.0 if i < s else 0.0
            mvec[:, 4 + i] = 1.0 if i > s else 0.0
        am = np.stack([m_prev, m_next, m_prev if s > 0 else m_none, m_next if s < 3 else m_none], axis=1)
        am = np.repeat(am[:, :, None, :], 4, axis=2).reshape(128, 4 * 512)
        m = dict(shared)
        m.update(xe=xe, ctxb=np.ascontiguousarray(ctx[b]), cfm=cfm, vmask=vmask, mvec=mvec,
                 amask=am.astype(ml_dtypes.bfloat16), rope=rope.astype(ml_dtypes.bfloat16))
        maps.append(m)
    return maps


_NC_CACHE = {}


def kernel(**inputs):
    maps = prep_inputs(inputs)
    if "nc" not in _NC_CACHE:
        _NC_CACHE["nc"] = build_nc()
    res = run_bass_kernel_spmd(_NC_CACHE["nc"], maps, core_ids=list(range(NCORES)))
    outp = np.zeros((2, 8192, D), np.float32)
    for r in range(NCORES):
        b, s = r // 4, r % 4
        outp[b, s * T:(s + 1) * T] = res.results[r]["out"]
    return outp
```

```python
from contextlib import ExitStack
import numpy as np
import ml_dtypes
import concourse.bass as bass
import concourse.mybir as mybir
from concourse.bass_utils import run_bass_kernel_spmd

F32 = mybir.dt.float32
BF16 = mybir.dt.bfloat16
AF = mybir.ActivationFunctionType
ALU = mybir.AluOpType
AX = mybir.AxisListType

NCORES = 8
D = 1024
T = 2048
NT = 16
NE = 32
EPS = 1e-6
NEG = -30000.0


class _Op:
    __slots__ = ("eng", "fn", "deps", "waits", "needed", "count", "dsem", "idx")


class Sched:
    def __init__(self, nc, es):
        self.nc, self.es = nc, es
        self.ops = []
        self.last_w, self.readers = {}, {}
        self.dma_sems, self.eng_sem = {}, {}
        self.alias = {}
        self.keyname = {}
        self.alias_done = set()

    def _sem(self, name):
        return self.es.enter_context(self.nc.semaphore(name))

    def op(self, eng, fn, reads=(), writes=(), dsem=None):
        o = _Op()
        o.eng, o.fn, o.dsem, o.needed, o.count, o.idx = eng, fn, dsem, False, None, len(self.ops)
        writes = list(writes)
        for k in list(writes):
            if k in self.alias_done:
                continue
            self.alias_done.add(k)
            for nm in (k, k[0] if isinstance(k, tuple) else None, self.keyname.get(k)):
                if nm is not None and nm in self.alias:
                    writes.extend(self.alias[nm])
        deps = set()
        for k in reads:
            w = self.last_w.get(k)
            if w is not None:
                deps.add(w)
        for k in writes:
            w = self.last_w.get(k)
            if w is not None and not (w.dsem is not None and dsem is not None):
                deps.add(w)
            deps.update(self.readers.get(k, ()))
        deps.discard(o)
        if eng == "tensor" and dsem is None:
            deps = {d for d in deps if not (d.eng == "tensor" and d.dsem is None)}
        o.deps = deps
        for k in writes:
            self.last_w[k] = o
            self.readers[k] = []
        for k in reads:
            self.readers.setdefault(k, []).append(o)
        self.ops.append(o)
        return o

    def finalize(self):
        for o in self.ops:
            for d in o.deps:
                d.needed = True
        cnt = {}
        for o in self.ops:
            if o.dsem is not None:
                if o.dsem not in self.dma_sems:
                    self.dma_sems[o.dsem] = self._sem("d_" + str(o.dsem))
                cnt[("d", o.dsem)] = cnt.get(("d", o.dsem), 0) + (1 if str(o.dsem).startswith("C:") else 16)
                o.count = cnt[("d", o.dsem)]
            elif o.needed:
                if o.eng not in self.eng_sem:
                    self.eng_sem[o.eng] = self._sem("e_" + o.eng)
                cnt[o.eng] = cnt.get(o.eng, 0) + 1
                o.count = cnt[o.eng]
        for o in self.ops:
            if o.dsem is not None and isinstance(o.dsem, str) and o.dsem.startswith("G:"):
                o.count = cnt[("d", o.dsem)]
        waited = {}
        for o in self.ops:
            need = {}
            for d in o.deps:
                sk = ("d", d.dsem) if d.dsem is not None else d.eng
                if d.count > need.get(sk, 0):
                    need[sk] = d.count
            o.waits = []
            for sk, v in need.items():
                if waited.get((o.eng, sk), 0) >= v:
                    continue
                waited[(o.eng, sk)] = v
                o.waits.append((sk, v))
        self.final_counts = cnt

    def emit(self, final_wait_eng="sync"):
        with self.nc.Block() as block:
            for eng in ("sync", "gpsimd", "scalar", "vector", "tensor"):
                ops = [o for o in self.ops if o.eng == eng]
                last = eng == final_wait_eng

                def body(e, ops=ops, last=last):
                    for o in ops:
                        for sk, v in o.waits:
                            s = self.dma_sems[sk[1]] if isinstance(sk, tuple) else self.eng_sem[sk]
                            e.wait_ge(s, v)
                        inst = o.fn(e)
                        if o.dsem is not None and str(o.dsem).startswith("C:"):
                            inst.then_inc(self.dma_sems[o.dsem])
                        elif o.dsem is not None:
                            inst.then_inc(self.dma_sems[o.dsem], 16)
                        elif o.needed:
                            inst.then_inc(self.eng_sem[o.eng], 1)
                    if last:
                        for sk, v in self.final_counts.items():
                            s = self.dma_sems[sk[1]] if isinstance(sk, tuple) else self.eng_sem[sk]
                            e.wait_ge(s, v)

                getattr(block, eng)(body)


def build_nc(debug=(), stop=None):
    nc = bass.Bass("TRN2", target_bir_lowering=False)

    def din(name, shape, dt=F32):
        return nc.dram_tensor(name, list(shape), dt, kind="ExternalInput").ap()

    xe = din("xe", [2304, D])
    ctxb = din("ctxb", [256, D])
    cfm = din("cfm", [128, 16])
    vmask = din("vmask", [128, 4])
    mvec = din("mvec", [128, 8])
    amask_d = din("amask", [128, 4 * 512], BF16)
    rope_d = din("rope", [128, 2 * 2304], BF16)
    wfm = din("wfm", [20, 128, 8 * 128])
    wvd_d = din("wvd", [128, 8 * 144])
    wz_d = din("wz", [128, 8 * 512])
    w_ada = din("w_ada", [D, 6 * D])
    b_ada = din("b_ada", [6 * D])
    norm1 = din("norm1", [D])
    convw_d = din("convw", [128, 40])
    convb_d = din("convb", [128, 8])
    dtb_d = din("dtb", [16])
    alog_d = din("alog", [16])
    dskip_d = din("dskip", [8])
    ssdn_d = din("ssdnorm", [512])
    sinks_d = din("sinks", [8])
    w_out = din("w_out", [D, D])
    norm2 = din("norm2", [D])
    wr_d = din("wr", [128, 8 * 36])
    br_d = din("br", [36])
    ne_decl = NE if stop is None else 1
    w_gate = din("w_gate", [ne_decl, D, 512])
    w_up = din("w_up", [ne_decl, D, 512])
    w_down = din("w_down", [ne_decl, 512, D])
    normf = din("normf", [D])
    out = nc.dram_tensor("out", [T, D], F32, kind="ExternalOutput").ap()
    xin = nc.dram_tensor("xin", [128, 1040], F32)
    xout = nc.dram_tensor("xout", [4 * 128, 1040], F32)
    CAP = 512
    Xbuf = nc.dram_tensor("Xbuf", [NE * CAP, D], BF16)
    Ybuf = nc.dram_tensor("Ybuf", [NE * CAP, D], BF16)
    dbg = {}
    for name, shape, dt_ in debug:
        dbg[name] = nc.dram_tensor("dbg_" + name, list(shape), dt_, kind="ExternalOutput").ap()

    es = ExitStack()
    with es:
        S = Sched(nc, es)
        ARENA_W = 52736
        arena = es.enter_context(nc.sbuf_tensor("arena", [128, ARENA_W], F32))
        banks = [es.enter_context(nc.psum_tensor(f"ps{i}", [128, 512], F32)) for i in range(8)]

        def PB(i):
            return banks[i][:, :]

        def PBh(i):
            return banks[i][:, :].bitcast(BF16)

        class Region:
            def __init__(self, base_kib, size_kib):
                self.base = int(base_kib * 256)
                self.size = int(size_kib * 256)
                self.off = 0

            def reset(self):
                self.off = 0

            def get(self, nelem, dt=F32, name=None):
                words = (nelem * (2 if dt == BF16 else 4) + 3) // 4
                words = (words + 7) // 8 * 8
                a = self.base + self.off
                self.off += words
                assert self.off <= self.size, (name, self.off, self.size)
                if name is not None:
                    olds = [k for (lo, hi, k) in ALLOCS if lo < a + words and a < hi and k != name]
                    if olds:
                        S.alias.setdefault(name, [])
                        for k in olds:
                            S.alias[name].extend(ALLKEYS.get(k, [k]))
                    ALLOCS.append((a, a + words, name))
                    S.alias_done = {k for k in S.alias_done
                                    if not (k == name or (isinstance(k, tuple) and k[0] == name) or S.keyname.get(k) == name)}
                ap = arena[:, a:a + words]
                if dt == BF16:
                    ap = ap.bitcast(BF16)
                return ap[:, 0:nelem]

        ALLOCS = []
        ALLKEYS = {}

        def keys(name, n):
            ks = [(name, i) for i in range(n)]
            ALLKEYS[name] = ks
            return ks

        R_const = Region(0, 4)
        R_vec = Region(4, 6)
        R_mod2 = Region(10, 12)
        R_A = Region(22, 32)
        R_B = Region(54, 16)
        R_C = Region(70, 16)
        R_hx = Region(86, 40)
        R_mod1 = Region(126, 20)
        R_dt = Region(146, 8)
        R_BC = Region(154, 17)
        R_tm = Region(171, 27)
        R_x = Region(154, 44)
        R_late = Region(126, 72)
        R_misc = Region(198, 8)

        def dma(eng, sem, out_, in_, r=(), w=()):
            S.op(eng, lambda e: e.dma_start(out=out_, in_=in_), r, w, dsem=sem)

        def tt(eng, out_, in0, in1, op, r=(), w=()):
            S.op(eng, lambda e: e.tensor_tensor(out=out_, in0=in0, in1=in1, op=op), r, w)

        def ts(eng, out_, in0, s1, s2, op0, op1=None, r=(), w=()):
            if op1 is None:
                S.op(eng, lambda e: e.tensor_scalar(out=out_, in0=in0, scalar1=s1, scalar2=None, op0=op0), r, w)
            else:
                S.op(eng, lambda e: e.tensor_scalar(out=out_, in0=in0, scalar1=s1, scalar2=s2, op0=op0, op1=op1), r, w)

        def stt(out_, in0, sc, in1, op0, op1, r=(), w=()):
            S.op("vector", lambda e: e.scalar_tensor_tensor(out=out_, in0=in0, scalar=sc, in1=in1, op0=op0, op1=op1), r, w)

        def act(out_, in_, func, r=(), w=(), bias=None, scale=None, accum=None):
            kw = {}
            if bias is not None:
                kw["bias"] = bias
            if scale is not None:
                kw["scale"] = scale
            if accum is not None:
                kw["accum_out"] = accum
            S.op("scalar", lambda e: e.activation(out=out_, in_=in_, func=func, **kw), r, w)

        def mm(out_, lhsT, rhs, start, stop, r=(), w=()):
            S.op("tensor", lambda e: e.matmul(out_, lhsT=lhsT, rhs=rhs, start=start, stop=stop, skip_group_check=True), r, w)

        def tr(out_, in_, ident, r=(), w=()):
            S.op("tensor", lambda e: e.transpose(out=out_, in_=in_, identity=ident), r, w)

        def cp(eng, out_, in_, r=(), w=()):
            if eng == "scalar":
                S.op(eng, lambda e: e.copy(out=out_, in_=in_), r, w)
            else:
                S.op(eng, lambda e: e.tensor_copy(out=out_, in_=in_), r, w)

        def red(out_, in_, op, r=(), w=()):
            S.op("vector", lambda e: e.tensor_reduce(out=out_, in_=in_, axis=AX.X, op=op), r, w)

        def recip(out_, in_, r=(), w=()):
            S.op("vector", lambda e: e.reciprocal(out=out_, in_=in_), r, w)

        def mset(eng, ap, val, w=()):
            S.op(eng, lambda e: e.memset(ap, val), (), w)

        def asel(ap, pattern, cmp, base, cm, key):
            S.op("gpsimd", lambda e: e.affine_select(out=ap, in_=ap, pattern=pattern, compare_op=cmp, fill=0.0,
                                                     base=base, channel_multiplier=cm), [key], [key])

        def tap(name, src, r):
            if name in dbg:
                dma("sync", "dbg", dbg[name], src, r=r)

        def v3(ap, a):
            return ap.rearrange("p (a b) -> p a b", a=a)

        def bc(ap, shape):
            return ap.to_broadcast(list(shape))

        def finish():
            S.finalize()
            S.emit()

        identf = R_const.get(128)
        identb = R_const.get(128, BF16)
        LE = R_const.get(128)
        GE = R_const.get(128)
        GT = R_const.get(128)
        LT = R_const.get(128)
        onesf = R_const.get(128)
        mset("gpsimd", identf, 1.0, w=["identf"])
        asel(identf, [[-1, 128]], ALU.is_equal, 0, 1, "identf")
        cp("vector", identb, identf, r=["identf"], w=["identb"])
        mset("gpsimd", onesf, 1.0, w=["onesf"])
        for ap_, key, pat, cmpop, base, cm in (
            (LE, "LE", [[1, 128]], ALU.is_ge, 0, -1),
            (GE, "GE", [[-1, 128]], ALU.is_ge, 0, 1),
            (GT, "GT", [[-1, 128]], ALU.is_gt, 0, 1),
            (LT, "LT", [[1, 128]], ALU.is_gt, 0, -1),
        ):
            mset("gpsimd", ap_, 1.0, w=[key])
            asel(ap_, pat, cmpop, base, cm, key)

        zt = R_misc.get(1024, BF16)
        mset("gpsimd", zt, 0.0, w=["zt"])

        convw = R_vec.get(40)
        convb = R_vec.get(8)
        dtb = R_vec.get(16)
        aneg = R_vec.get(16)
        d8 = R_vec.get(8)
        D512 = R_vec.get(512)
        ssdn = R_vec.get(512)
        esink = R_vec.get(8)
        vm = R_vec.get(4)
        mv = R_vec.get(8)
        brb = R_vec.get(36)
        wr = R_vec.get(288)
        dma("gpsimd", "G:vec", convw, convw_d[:, :], w=["convw"])
        dma("gpsimd", "G:vec", convb, convb_d[:, :], w=["convb"])
        dma("gpsimd", "G:vec", dtb, dtb_d.partition_broadcast(128), w=["dtb"])
        dma("gpsimd", "G:vec", aneg, alog_d.partition_broadcast(128), w=["aneg"])
        dma("gpsimd", "G:vec", d8, dskip_d.partition_broadcast(128), w=["d8"])
        dma("gpsimd", "G:vec", ssdn, ssdn_d.partition_broadcast(128), w=["ssdn"])
        dma("gpsimd", "G:vec", esink, sinks_d.partition_broadcast(128), w=["esink"])
        dma("gpsimd", "G:vec", vm, vmask[:, :], w=["vm"])
        dma("gpsimd", "G:vec", mv, mvec[:, :], w=["mv"])
        dma("gpsimd", "G:vec", brb, br_d.partition_broadcast(128), w=["brb"])
        dma("gpsimd", "G:vec", wr, wr_d[:, :], w=["wr"])
        act(aneg, aneg, AF.Exp, r=["aneg"], w=["aneg"])
        ts("vector", aneg, aneg, -1.0, None, ALU.mult, r=["aneg"], w=["aneg"])
        act(esink, esink, AF.Exp, r=["esink"], w=["esink"])
        cp("vector", v3(D512, 8), bc(d8.unsqueeze(2), [128, 8, 64]), r=["d8"], w=["D512"])

        sh2 = R_mod2.get(D)
        gsc2 = R_mod2.get(D)
        g2 = R_mod2.get(D)
        sh1 = R_mod1.get(D, name="sh1")
        gsc1 = R_mod1.get(D, name="gsc1")
        g1 = R_mod1.get(D, name="g1")
        csh1 = R_mod1.get(D, name="csh1")
        cgsc1 = R_mod1.get(D, name="cgsc1")
        modx = [sh1, gsc1, g1, sh2, gsc2, g2]
        modc = [csh1, cgsc1]
        modx_k = ["sh1", "gsc1", "g1", "sh2", "gsc2", "g2"]
        modc_k = ["csh1", "cgsc1"]
        for k_ in modx_k + modc_k:
            keys(k_, 2)

        scf = R_tm.get(16, name="scf")
        Lrep = R_tm.get(16 * 128, name="Lrep")
        n1b = R_tm.get(D, name="n1b")
        n2b = R_tm.get(D, name="n2b")
        dma("sync", "scf", scf, cfm[:, :], w=["scf"])
        dma("sync", "G:nb", n1b, norm1.partition_broadcast(128), w=["n1b"])
        dma("sync", "G:nb", n2b, norm2.partition_broadcast(128), w=["n2b"])
        act(scf, scf, AF.Silu, r=["scf"], w=["scf"])
        cp("vector", v3(Lrep, 16), bc(scf.unsqueeze(2), [128, 16, 128]), r=["scf"], w=["Lrep"])
        Lr = v3(Lrep, 16)
        keys("wst", 2)
        keys("bst", 2)
        wst = [R_A.get(8 * 512, name="wst"), R_A.get(8 * 512, name="wst")]
        bst = [R_B.get(512, name="bst"), R_B.get(512, name="bst")]
        w_ada_v = w_ada.rearrange("(kc p) n -> p kc n", p=128)
        for j in range(12):
            sl = j % 2
            dma("sync", ("wst", sl), v3(wst[sl], 8), w_ada_v[:, :, j * 512:(j + 1) * 512], w=[("wst", sl)])
            dma("gpsimd", ("bst", sl), bst[sl], b_ada[j * 512:(j + 1) * 512].partition_broadcast(128), w=[("bst", sl)])
            variants = [(0, modx[j // 2], modx_k[j // 2], sl)]
            if j < 4:
                variants.append((1, modc[j // 2], modc_k[j // 2], 2 + sl))
            for vi, dst, dk, bank in variants:
                for kc in range(8):
                    mm(PB(bank), Lr[:, vi * 8 + kc, :], v3(wst[sl], 8)[:, kc, :], kc == 0, kc == 7,
                       r=["Lrep", ("wst", sl)], w=[("ps", bank)])
                tt("vector", dst[:, (j % 2) * 512:(j % 2 + 1) * 512], PB(bank), bst[sl], ALU.add,
                   r=[("ps", bank), ("bst", sl)], w=[(dk, j % 2)])
        for dst, dk, nb, nk in ((gsc1, "gsc1", n1b, "n1b"), (cgsc1, "cgsc1", n1b, "n1b"), (gsc2, "gsc2", n2b, "n2b")):
            stt(dst, dst, 1.0, nb, ALU.add, ALU.mult, r=[(dk, 0), (dk, 1), nk], w=[(dk, 0), (dk, 1)])
        tap("sh1", sh1, [("sh1", 0), ("sh1", 1)])
        tap("gsc1", gsc1, [("gsc1", 0), ("gsc1", 1)])
        tap("g2", g2, [("g2", 0), ("g2", 1)])

        keys("hxT", 20)
        hxT = v3(R_hx.get(8 * 2560, BF16, name="hxT"), 8)
        keys("xst", 3)
        xst = [R_C.get(D, name="xst") for _ in range(3)]
        keys("hxb", 2)
        hxb = [R_B.get(D, BF16, name="hxb") for _ in range(2)]
        junk = R_B.get(D, BF16, name="junk")
        hxf = R_B.get(D, name="hxf")
        ssq = R_dt.get(32)
        keys("ssq", 20)
        for t in range(20):
            sl = t % 3
            src = xe[t * 128:(t + 1) * 128, :] if t < 18 else ctxb[(t - 18) * 128:(t - 17) * 128, :]
            dma("sync", ("xst", sl), xst[sl], src, w=[("xst", sl)])
            act(junk, xst[sl], AF.Square, r=[("xst", sl)], w=["junk", ("ssq", t)], accum=ssq[:, t:t + 1])
            act(ssq[:, t:t + 1], ssq[:, t:t + 1], AF.Sqrt, r=[("ssq", t)], w=[("ssq", t)], bias=EPS, scale=1.0 / D)
            recip(ssq[:, t:t + 1], ssq[:, t:t + 1], r=[("ssq", t)], w=[("ssq", t)])
            gs, gk, sh, sk = (gsc1, "gsc1", sh1, "sh1") if t < 18 else (cgsc1, "cgsc1", csh1, "csh1")
            stt(hxf, xst[sl], ssq[:, t:t + 1], gs, ALU.mult, ALU.mult,
                r=[("xst", sl), ("ssq", t), (gk, 0), (gk, 1)], w=["hxf"])
            hs = t % 2
            tt("vector", hxb[hs], hxf, sh, ALU.add, r=["hxf", (sk, 0), (sk, 1)], w=[("hxb", hs)])
            if t in (0, 17):
                c = 0 if t == 0 else 1
                ts("vector", hxb[hs], hxb[hs], vm[:, c:c + 1], None, ALU.mult, r=[("hxb", hs), "vm"], w=[("hxb", hs)])
            bank = t % 2
            for kc in range(8):
                tr(PBh(bank)[:, kc * 128:(kc + 1) * 128], hxb[hs][:, kc * 128:(kc + 1) * 128], identb,
                   r=[("hxb", hs), "identb"], w=[("ps", bank)])
            cp("scalar", hxT[:, :, t * 128:(t + 1) * 128], v3(PBh(bank)[:, 0:1024], 8), r=[("ps", bank)], w=[("hxT", t)])
        for kc_ in range(8):
            tap("hxT%d" % kc_, hxT[:, kc_, :], [("hxT", t) for t in range(20)])
        if stop == "B":
            finish()
            return nc

        R_A.reset()
        qT = v3(R_A.get(4 * 2048, BF16, name="qT"), 4)
        kTd = v3(R_A.get(2 * 2304, BF16, name="kTd"), 2)
        vaug_f = R_A.get(20 * 2 * 66, BF16, name="vaug")
        vaug = vaug_f.rearrange("p (t h c) -> p t h c", t=20, h=2)
        kcT = v3(R_A.get(2 * 256, BF16, name="kcT"), 2)
        keys("qT", 16)
        keys("kTd", 18)
        keys("vaug", 20)
        keys("kcT", 2)
        amask = R_x.get(4 * 512, BF16, name="amask")
        ropeT = R_x.get(2 * 2304, BF16, name="ropeT")
        cosT, sinT = ropeT[:, 0:2304], ropeT[:, 2304:4608]
        keys("PT", 5)
        PT = [R_x.get(512, BF16, name="PT") for _ in range(5)]
        keys("rt", 4)
        rt = [R_x.get(512, name="rt") for _ in range(4)]
        keys("wsf", 2)
        wsf_all = R_x.get(2048, name="wsf")
        wsf = [wsf_all[:, 0:1024], wsf_all[:, 1024:2048]]
        keys("wsb", 4)
        wsb_all = R_x.get(4096, BF16, name="wsb")
        wsb = [wsb_all[:, i * 1024:(i + 1) * 1024] for i in range(4)]
        dtraw_f = R_misc.get(20 * 16)
        dtraw = v3(dtraw_f, 20)
        keys("dtraw", 20)
        dma("gpsimd", "amask", amask, amask_d[:, :], w=["amask"])
        dma("gpsimd", "ropeT", ropeT, rope_d[:, :], w=["ropeT"])
        mset("vector", dtraw_f, 0.0, w=keys("dtraw", 20))
        wvdf = wsf_all[:, 0:1152]
        wvdb = wsb_all[:, 0:1152]
        dma("sync", ("wsf", 0), wvdf, wvd_d[:, :], w=[("wsf", 0), ("wsf", 1)])
        cp("vector", wvdb, wvdf, r=[("wsf", 0), ("wsf", 1)], w=[("wsb", 0), ("wsb", 1)])
        mset("gpsimd", vaug_f, 1.0, w=keys("vaug", 20))
        for t in range(20):
            bank = 6 + t % 2
            for kc in range(8):
                mm(PB(bank)[:, 0:144], hxT[:, kc, t * 128:(t + 1) * 128], v3(wvdb, 8)[:, kc, :], kc == 0, kc == 7,
                   r=[("hxT", t), ("wsb", 0), ("wsb", 1)], w=[("ps", bank)])
            cp("scalar", vaug[:, t, :, 0:64], PB(bank)[:, 0:128].rearrange("p (a b) -> p a b", a=2),
               r=[("ps", bank)], w=[("vaug", t)])
            if 1 <= t <= 16 or t >= 18:
                cp("scalar", dtraw[:, t, :], PB(bank)[:, 128:144], r=[("ps", bank)], w=[("dtraw", t)])
        if stop == "C0":
            tap("vaug", vaug_f, [("vaug", i) for i in range(20)])
            finish()
            return nc
        wcnt = [0]

        def load_w(ci, slot_b):
            sf = wcnt[0] % 2
            wcnt[0] += 1
            dma("sync", ("wsf", sf), wsf[sf], wfm[ci], w=[("wsf", sf)])
            cp("gpsimd", wsb[slot_b], wsf[sf], r=[("wsf", sf)], w=[("wsb", slot_b)])

        gcnt = [0]

        def proj_pair(sa, sb, e0, n, dst, dkeys, do_rope=True):
            i = gcnt[0] % 2
            gcnt[0] += 1
            a, b_ = 2 + 2 * i, 3 + 2 * i
            hk_ = [("hxT", t) for t in range(e0 // 128, (e0 + n + 127) // 128)]
            for kc in range(8):
                mm(PB(a)[:, 0:n], v3(wsb[sa], 8)[:, kc, :], hxT[:, kc, e0:e0 + n], kc == 0, kc == 7,
                   r=hk_ + [("wsb", sa)], w=[("ps", a)])
            if not do_rope:
                cp("scalar", dst, PB(a)[:, 0:n], r=[("ps", a)], w=dkeys)
                return
            for kc in range(8):
                mm(PB(b_)[:, 0:n], v3(wsb[sb], 8)[:, kc, :], hxT[:, kc, e0:e0 + n], kc == 0, kc == 7,
                   r=hk_ + [("wsb", sb)], w=[("ps", b_)])
            tt("vector", rt[2 * i][:, 0:n], PB(a)[:, 0:n], cosT[:, e0:e0 + n], ALU.mult, r=[("ps", a), "ropeT"], w=[("rt", 2 * i)])
            tt("vector", rt[2 * i + 1][:, 0:n], PB(b_)[:, 0:n], sinT[:, e0:e0 + n], ALU.mult, r=[("ps", b_), "ropeT"], w=[("rt", 2 * i + 1)])
            tt("gpsimd", dst, rt[2 * i][:, 0:n], rt[2 * i + 1][:, 0:n], ALU.add, r=[("rt", 2 * i), ("rt", 2 * i + 1)], w=dkeys)

        for c in range(4):
            sa, sb = 2 * (c % 2), 2 * (c % 2) + 1
            load_w(8 + c, sa)
            load_w(12 + c, sb)
            for g in range(4):
                proj_pair(sa, sb, 128 + 512 * g, 512, qT[:, c, 512 * g:512 * (g + 1)], [("qT", 4 * g + i) for i in range(4)])
        for hk in range(2):
            sa, sb = 2 * (hk % 2), 2 * (hk % 2) + 1
            load_w(16 + hk, sa)
            load_w(18 + hk, sb)
            for g in range(5):
                n = 512 if g < 4 else 256
                proj_pair(sa, sb, 512 * g, n, kTd[:, hk, 512 * g:512 * g + n], [("kTd", 4 * g + i) for i in range(n // 128)])
            proj_pair(sa, sb, 2304, 256, kcT[:, hk, :], [("kcT", hk)], do_rope=False)
        for c_ in range(4):
            tap("qT%d" % c_, qT[:, c_, :], [("qT", i) for i in range(16)])
        for c_ in range(2):
            tap("kTd%d" % c_, kTd[:, c_, :], [("kTd", i) for i in range(18)])
            tap("kcT%d" % c_, kcT[:, c_, :], [("kcT", c_)])
        tap("vaug", vaug_f, [("vaug", i) for i in range(20)])
        if stop == "C1":
            finish()
            return nc

        R_C.reset()
        attn_all = R_C.get(16 * 256, name="attn")
        attn = v3(attn_all.bitcast(BF16), 16)
        keys("attn", 16)
        den = R_misc.get(8)
        R_B.reset()
        keys("qz", 4)
        qz_all = R_B.get(4 * 512, BF16, name="qz")
        qz = [[v3(qz_all[:, (2 * b_ + h_) * 512:(2 * b_ + h_ + 1) * 512], 4) for h_ in range(2)] for b_ in range(2)]
        mset("gpsimd", qz_all, 0.0, w=keys("qz", 4))
        scnt = [0]
        for n in range(NT):
            t = n + 1
            qb = n % 2
            cp("gpsimd", qz[qb][0][0:64, :, :], qT[0:64, :, n * 128:(n + 1) * 128], r=[("qT", n)], w=[("qz", 2 * qb)])
            cp("gpsimd", qz[qb][1][64:128, :, :], qT[64:128, :, n * 128:(n + 1) * 128], r=[("qT", n)], w=[("qz", 2 * qb + 1)])
            for hk in range(2):
                ktiles = [("k", t - 1, 2 if n == 0 else 0), ("k", t, None), ("k", t + 1, 3 if n == NT - 1 else 1),
                          ("c", 0, None), ("c", 1, None)]
                obank = 6 + (2 * n + hk) % 2
                pts = []
                for kind, idx, mtype in ktiles:
                    sb_ = scnt[0] % 6
                    ps_ = scnt[0] % 5
                    scnt[0] += 1
                    if mtype is not None:
                        mm(PB(sb_), identb, amask[:, mtype * 512:(mtype + 1) * 512], True, False,
                           r=["identb", "amask"], w=[("ps", sb_)])
                    for i in range(4):
                        h = 4 * hk + i
                        ch, half = h // 2, h % 2
                        if kind == "k":
                            lhs = kTd[:, hk, idx * 128:(idx + 1) * 128]
                            rk = [("kTd", idx)]
                        else:
                            lhs = kcT[:, hk, idx * 128:(idx + 1) * 128]
                            rk = [("kcT", hk)]
                        mm(PB(sb_)[:, i * 128:(i + 1) * 128], lhs, qz[qb][half][:, ch, :],
                           mtype is None, True, r=rk + [("qz", 2 * qb + half)], w=[("ps", sb_)])
                    act(PT[ps_], PB(sb_), AF.Exp, r=[("ps", sb_)], w=[("PT", ps_)], scale=0.125)
                    pts.append((ps_, kind, idx))
                ob = PB(obank)[:, 0:260].rearrange("p (a b) -> p a b", a=4)
                for i in range(4):
                    for j, (ps_, kind, idx) in enumerate(pts):
                        vt = idx if kind == "k" else 18 + idx
                        mm(ob[:, i, :], PT[ps_][:, i * 128:(i + 1) * 128], vaug[:, vt, hk, 0:65], j == 0, j == 4,
                           r=[("PT", ps_), ("vaug", vt)], w=[("ps", obank)])
                dn = den[:, 4 * hk:4 * hk + 4]
                tt("vector", dn, ob[:, :, 64], esink[:, 4 * hk:4 * hk + 4], ALU.add, r=[("ps", obank), "esink"], w=[("den", hk)])
                recip(dn, dn, r=[("den", hk)], w=[("den", hk)])
                tt("vector", attn[:, n, 256 * hk:256 * (hk + 1)].rearrange("p (a b) -> p a b", a=4), ob[:, :, 0:64],
                   bc(dn.unsqueeze(2), [128, 4, 64]), ALU.mult, r=[("ps", obank), ("den", hk)], w=[("attn", n)])
        tap("attn", attn_all.bitcast(BF16), [("attn", i) for i in range(16)])
        if stop == "C":
            finish()
            return nc

        R_A.reset()
        keys("wsf", 2)
        wsf_all = R_A.get(2048, name="wsf")
        wsf = [wsf_all[:, 0:1024], wsf_all[:, 1024:2048]]
        keys("wsb", 4)
        wsb_all = R_A.get(4096, BF16, name="wsb")
        wsb = [wsb_all[:, i * 1024:(i + 1) * 1024] for i in range(4)]
        keys("cacc", 2)
        cacc = [R_A.get(512, name="cacc") for _ in range(2)]
        keys("xsTr", 2)
        xsTr = [R_A.get(2048, BF16, name="xsTr") for _ in range(2)]
        xsTc = R_A.get(256, BF16, name="xsTc")
        R_m1b = Region(138, 8)
        keys("wzb", 4)
        wzb = v3(R_m1b.get(8 * 512, BF16, name="wzb"), 8)
        R_B.reset()
        keys("siluz", 16)
        siluz_all = R_B.get(16 * 256, name="siluz")
        siluz = v3(siluz_all.bitcast(BF16), 16)
        R_BC.reset()
        keys("BT", 2)
        keys("BTc", 2)
        keys("xs_tm_c", 1)
        keys("B_tm_c", 1)
        keys("CT", 2)
        BT = v3(R_BC.get(2 * 2048, BF16, name="BT"), 2)
        CT = v3(R_BC.get(2 * 2048, BF16, name="CT"), 2)
        BTc = v3(R_BC.get(2 * 256, BF16, name="BTc"), 2)
        R_tm.reset()
        keys("xs_tm", 16)
        keys("B_tm", 16)
        xs_tm = v3(R_tm.get(16 * 512, BF16, name="xs_tm"), 16)
        B_tm = v3(R_tm.get(16 * 256, BF16, name="B_tm"), 16)
        xs_tm_c = v3(R_tm.get(2 * 512, BF16, name="xs_tm_c"), 2)
        B_tm_c = v3(R_tm.get(2 * 256, BF16, name="B_tm_c"), 2)

        for piece in range(4):
            sf = wcnt[0] % 2
            wcnt[0] += 1
            dma("sync", ("wsf", sf), wsf[sf], wz_d[:, piece * 1024:(piece + 1) * 1024], w=[("wsf", sf)])
            cp("gpsimd", wzb[:, 2 * piece:2 * piece + 2, :], v3(wsf[sf], 2), r=[("wsf", sf)], w=[("wzb", piece)])
        for n in range(NT):
            t = n + 1
            bank = 6 + n % 2
            for kc in range(8):
                mm(PB(bank), hxT[:, kc, t * 128:(t + 1) * 128], wzb[:, kc, :], kc == 0, kc == 7,
                   r=[("hxT", t), ("wzb", kc // 2)], w=[("ps", bank)])
            act(siluz[:, n, :], PB(bank), AF.Silu, r=[("ps", bank)], w=[("siluz", n)])

        ccnt = [0]

        def conv_window(j, sbw, e0, nn, Lo, dst, dk, ctxw):
            bank = 2 + ccnt[0] % 4
            sl = ccnt[0] % 2
            ccnt[0] += 1
            hk_ = [("hxT", t) for t in range(e0 // 128, (e0 + nn - 1) // 128 + 1)]
            for kc in range(8):
                mm(PB(bank)[:, 0:nn], v3(wsb[sbw], 8)[:, kc, :], hxT[:, kc, e0:e0 + nn], kc == 0, kc == 7,
                   r=hk_ + [("wsb", sbw)], w=[("ps", bank)])
            wk = lambda k: convw[:, j * 5 + k:j * 5 + k + 1]
            rr = [("ps", bank), ("cacc", sl), "convw", "convb"]
            if not ctxw:
                ts("vector", cacc[sl][:, 0:Lo], PB(bank)[:, 0:Lo], wk(0), convb[:, j:j + 1], ALU.mult, ALU.add, r=rr, w=[("cacc", sl)])
                for k in range(1, 5):
                    stt(cacc[sl][:, 0:Lo], PB(bank)[:, k:k + Lo], wk(k), cacc[sl][:, 0:Lo], ALU.mult, ALU.add, r=rr, w=[("cacc", sl)])
            else:
                ts("vector", cacc[sl][:, 0:256], PB(bank)[:, 0:256], wk(2), convb[:, j:j + 1], ALU.mult, ALU.add, r=rr, w=[("cacc", sl)])
                for k, (olo, ohi, ilo) in ((0, (2, 256, 0)), (1, (1, 256, 0)), (3, (0, 255, 1)), (4, (0, 254, 2))):
                    stt(cacc[sl][:, olo:ohi], PB(bank)[:, ilo:ilo + (ohi - olo)], wk(k), cacc[sl][:, olo:ohi], ALU.mult, ALU.add, r=rr, w=[("cacc", sl)])
            act(dst, cacc[sl][:, 0:Lo], AF.Silu, r=[("cacc", sl)], w=dk)

        for j in range(8):
            sbw = j % 4
            load_w(j, sbw)
            if j < 4:
                dfull, dk = xsTr[j % 2], [("xsTr", j % 2)]
            elif j < 6:
                dfull, dk = BT[:, j - 4, :], [("BT", j - 4)]
            else:
                dfull, dk = CT[:, j - 6, :], [("CT", j - 6)]
            for w_ in range(5):
                e0 = 126 + 508 * w_
                nn, Lo = (512, 508) if w_ < 4 else (20, 16)
                conv_window(j, sbw, e0, nn, Lo, dfull[:, 508 * w_:508 * w_ + Lo], dk, False)
            if j < 6:
                dc, dck = (xsTc, ["xsTc"]) if j < 4 else (BTc[:, j - 4, :], [("BTc", j - 4)])
                conv_window(j, sbw, 2304, 256, 256, dc, dck, True)
                if j < 4:
                    src, sk_, dst_t, dst_c, col0, dname = dfull, dk, xs_tm, xs_tm_c, j * 128, "xs_tm"
                else:
                    src, sk_, dst_t, dst_c, col0, dname = dfull, dk, B_tm, B_tm_c, (j - 4) * 128, "B_tm"
                for g8 in range(2):
                    bank = g8
                    for i in range(8):
                        tile = 8 * g8 + i
                        tr(PBh(bank)[:, i * 128:(i + 1) * 128], src[:, tile * 128:(tile + 1) * 128], identb,
                           r=sk_ + ["identb"], w=[("ps", bank)])
                    cp("scalar", dst_t[:, 8 * g8:8 * g8 + 8, col0:col0 + 128], v3(PBh(bank)[:, 0:1024], 8),
                       r=[("ps", bank)], w=[(dname, 8 * g8 + i) for i in range(8)])
                bank = 0
                for i in range(2):
                    tr(PBh(bank)[:, i * 128:(i + 1) * 128], dc[:, i * 128:(i + 1) * 128], identb, r=dck + ["identb"], w=[("ps", bank)])
                cp("scalar", dst_c[:, 0:2, col0:col0 + 128], v3(PBh(bank)[:, 0:256], 2), r=[("ps", bank)], w=[(dname + "_c", 0)])

        dts_f = R_misc.get(320)
        dA_f = R_misc.get(320)
        dts, dA = v3(dts_f, 20), v3(dA_f, 20)
        tt("vector", dts, dtraw, bc(dtb.unsqueeze(1), [128, 20, 16]), ALU.add, r=keys("dtraw", 20) + ["dtb"], w=["dts"])
        act(dts_f, dts_f, AF.Exp, r=["dts"], w=["dts"])
        act(dts_f, dts_f, AF.Ln, r=["dts"], w=["dts"], bias=1.0)
        tt("vector", dA, dts, bc(aneg.unsqueeze(1), [128, 20, 16]), ALU.mult, r=["dts", "aneg"], w=["dA"])
        tap("xs_tm", xs_tm.rearrange("p a b -> p (a b)"), keys("xs_tm", 16))
        tap("B_tm", B_tm.rearrange("p a b -> p (a b)"), keys("B_tm", 16))
        tap("CT", CT.rearrange("p a b -> p (a b)"), keys("CT", 2))
        tap("xs_tm_c", xs_tm_c.rearrange("p a b -> p (a b)"), [("xs_tm_c", 0)])
        tap("B_tm_c", B_tm_c.rearrange("p a b -> p (a b)"), [("B_tm_c", 0)])
        tap("dts", dts_f, ["dts"])
        tap("siluz", siluz_all.bitcast(BF16), keys("siluz", 16))
        if stop == "D":
            finish()
            return nc

        for i_ in range(NE * CAP // 128):
            dma("sync", "G:zf", Xbuf[i_ * 128:(i_ + 1) * 128, :], zt, r=["zt"], w=["Xzero"])
        R_A.reset()
        keys("yacc", 16)
        yacc_all = R_A.get(16 * 512, name="yacc")
        yacc = v3(yacc_all, 16)
        R_hx.reset()
        ST = R_hx.get(1040, name="ST")
        hTs = [ST[:, 0:512], ST[:, 512:1024]]
        cums = [ST[:, 1024:1032], ST[:, 1032:1040]]
        hC_all = R_hx.get(1024, name="hC")
        hCs = [hC_all[:, 0:512], hC_all[:, 512:1024]]
        hTb_all = R_hx.get(1024, BF16, name="hTb")
        hTbs = [hTb_all[:, 0:512], hTb_all[:, 512:1024]]
        cumC = R_hx.get(16, name="cumC")
        e_base = R_hx.off
        ALLKEYS["ST"] = ["hT0", "hT1", "cum0", "cum1"]
        ALLKEYS["hC"] = ["hC0", "hC1"]
        ALLKEYS["hTb"] = ["hTb0", "hTb1"]
        ALLKEYS["hin"] = ["hin0", "hin1"]
        for nm_ in ("ST", "hC", "hTb", "hin"):
            for k_ in ALLKEYS[nm_]:
                S.keyname[k_] = nm_
        for nm_ in ("TdA", "ES", "cbm", "MT", "dtx", "dtxw", "xsD", "tmpy", "sm", "smx"):
            keys(nm_, 2)
        TdA = [R_hx.get(1024, name="TdA") for _ in range(2)]
        ES = [R_hx.get(1024, BF16, name="ES") for _ in range(2)]
        cbm = [R_hx.get(256, BF16, name="cbm") for _ in range(2)]
        MT = [R_hx.get(1024, BF16, name="MT") for _ in range(2)]
        dtx = [R_hx.get(512, BF16, name="dtx") for _ in range(2)]
        dtxw = [R_hx.get(512, BF16, name="dtxw") for _ in range(2)]
        xsD = [R_hx.get(512, BF16, name="xsD") for _ in range(2)]
        tmpy = [R_hx.get(512, name="tmpy") for _ in range(2)]
        sm = [R_hx.get(16, name="sm") for _ in range(2)]
        smx_ = [R_hx.get(48, name="smx") for _ in range(2)]
        Epass_f = R_misc.get(16 * 16)
        Epass = Epass_f.rearrange("p (n d h) -> p n d h", n=16, d=2)
        mset("vector", ST, 0.0, w=["hT0", "hT1", "cum0", "cum1"])
        mset("vector", hC_all, 0.0, w=["hC0", "hC1"])
        mset("vector", cumC, 0.0, w=["cumC"])
        mset("gpsimd", hTb_all, 0.0, w=["hTb0", "hTb1"])
        ecnt = [0]

        def ssd_chunk(t, n, d, xs_src, xs_k, B_src, B_k, cols, hT, hk, hTb_d, hbk, cum_d, cumk, need_y):
            sl = ecnt[0] % 2
            Y_, YO_, ST_ = 3, 4, 5
            ecnt[0] += 1
            Tri, TriK = (LE, "LE") if d == 0 else (GE, "GE")
            Str, StrK = (GT, "GT") if d == 0 else (LT, "LT")
            dAc = dA[:, t, d * 8:(d + 1) * 8]
            dtc = dts[:, t, d * 8:(d + 1) * 8]
            wj, dtw, eat, eacs, ep = [smx_[sl][:, 8 * i:8 * i + 8] for i in range(5)]
            xs3 = xs_src.rearrange("p (h c) -> p h c", h=8)
            mm(PB(2)[:, 256:264], Tri, dAc, True, True, r=[TriK, "dA"], w=[("ps", 2)])
            mm(PB(2)[:, 264:272], onesf, dAc, True, True, r=["onesf", "dA"], w=[("ps", 2)])
            cp("vector", sm[sl], PB(2)[:, 256:272], r=[("ps", 2)], w=[("sm", sl)])
            acs, atot = sm[sl][:, 0:8], sm[sl][:, 8:16]
            sx = [("smx", sl)]
            tt("vector", wj, atot, acs, ALU.subtract, r=[("sm", sl)], w=sx)
            act(wj, wj, AF.Exp, r=sx, w=sx)
            tt("vector", dtw, wj, dtc, ALU.mult, r=sx + ["dts"], w=sx)
            act(eat, atot, AF.Exp, r=[("sm", sl)], w=sx)
            if need_y:
                act(eacs, acs, AF.Exp, r=[("sm", sl)], w=sx)
                tt("vector", ep, acs, cum_d, ALU.add, r=[("sm", sl), cumk], w=sx)
                act(Epass[:, n, d, :], ep, AF.Exp, r=sx, w=[("Ep", n, d)])
            tt("vector", cum_d, cum_d, atot, ALU.add, r=[cumk, ("sm", sl)], w=[cumk])
            if need_y:
                TdA3 = v3(TdA[sl], 8)
                tt("vector", TdA3, bc(Tri.unsqueeze(1), [128, 8, 128]), bc(dAc.unsqueeze(2), [128, 8, 128]), ALU.mult,
                   r=[TriK, "dA"], w=[("TdA", sl)])
                sb0 = 0 if sl == 0 else 6
                for hh in range(2):
                    mm(PB(sb0 + hh), Str, TdA[sl][:, hh * 512:(hh + 1) * 512], True, True, r=[StrK, ("TdA", sl)], w=[("ps", sb0 + hh)])
                for hh in range(2):
                    act(ES[sl][:, hh * 512:(hh + 1) * 512], PB(sb0 + hh), AF.Exp, r=[("ps", sb0 + hh)], w=[("ES", sl)])
                for g in range(2):
                    mm(PB(2)[:, g * 128:(g + 1) * 128], BT[:, g, cols], CT[:, g, cols], True, True,
                       r=[("BT", g), ("CT", g)], w=[("ps", 2)])
                cbm3 = v3(cbm[sl], 2)
                tt("vector", cbm3, v3(PB(2)[:, 0:256], 2), bc(Tri.unsqueeze(1), [128, 2, 128]), ALU.mult,
                   r=[("ps", 2), TriK], w=[("cbm", sl)])
                MT4 = MT[sl].rearrange("p (g r i) -> p g r i", g=2, r=4)
                ES4 = ES[sl].rearrange("p (g r i) -> p g r i", g=2, r=4)
                tt("vector", MT4, ES4, bc(cbm3.unsqueeze(2), [128, 2, 4, 128]), ALU.mult,
                   r=[("ES", sl), ("cbm", sl)], w=[("MT", sl)])
                dtx3 = v3(dtx[sl], 8)
                tt("vector", dtx3, xs3, bc(dtc.unsqueeze(2), [128, 8, 64]), ALU.mult, r=[xs_k, "dts"], w=[("dtx", sl)])
                if d == 0:
                    tt("gpsimd", xsD[sl], xs_src, D512, ALU.mult, r=[xs_k, "D512"], w=[("xsD", sl)])
                    mm(PB(Y_), identb, xsD[sl], True, False, r=["identb", ("xsD", sl)], w=[("ps", Y_)])
                MT3 = v3(MT[sl], 8)
                for h in range(8):
                    mm(PB(Y_)[:, h * 64:(h + 1) * 64], MT3[:, h, :], dtx3[:, h, :], d != 0, True,
                       r=[("MT", sl), ("dtx", sl)], w=[("ps", Y_)])
                for g in range(2):
                    mm(PB(YO_)[:, g * 256:(g + 1) * 256], CT[:, g, cols], hTb_d[:, g * 256:(g + 1) * 256], True, True,
                       r=[("CT", g), hbk], w=[("ps", YO_)])
                tt("vector", v3(tmpy[sl], 8), v3(PB(YO_), 8), bc(eacs.unsqueeze(2), [128, 8, 64]), ALU.mult,
                   r=[("ps", YO_)] + sx, w=[("tmpy", sl)])
                if d == 0:
                    tt("vector", yacc[:, n, :], tmpy[sl], PB(Y_), ALU.add, r=[("tmpy", sl), ("ps", Y_)], w=[("yacc", n)])
                else:
                    tt("gpsimd", yacc[:, n, :], yacc[:, n, :], tmpy[sl], ALU.add, r=[("tmpy", sl), ("yacc", n)], w=[("yacc", n)])
                    tt("vector", yacc[:, n, :], yacc[:, n, :], PB(Y_), ALU.add, r=[("ps", Y_), ("yacc", n)], w=[("yacc", n)])
            dtxw3 = v3(dtxw[sl], 8)
            tt("vector", dtxw3, xs3, bc(dtw.unsqueeze(2), [128, 8, 64]), ALU.mult, r=[xs_k] + sx, w=[("dtxw", sl)])
            for g in range(2):
                mm(PB(ST_)[:, g * 256:(g + 1) * 256], B_src[:, g * 128:(g + 1) * 128], dtxw[sl][:, g * 256:(g + 1) * 256],
                   True, True, r=[B_k, ("dtxw", sl)], w=[("ps", ST_)])
            tt("vector", v3(hT, 8), v3(hT, 8), bc(eat.unsqueeze(2), [128, 8, 64]), ALU.mult, r=[hk] + sx, w=[hk])
            tt("vector", hT, hT, PB(ST_), ALU.add, r=[hk, ("ps", ST_)], w=[hk])
            if hTb_d is not None:
                cp("scalar", hTb_d, hT, r=[hk], w=[hbk])

        for ci in (0, 1):
            ssd_chunk(18 + ci, None, 0, xs_tm_c[:, ci, :], ("xs_tm_c", 0), B_tm_c[:, ci, :], ("B_tm_c", 0), None,
                      hCs[0], "hC0", None, None, cumC[:, 0:8], "cumC", False)
        for ci in (1, 0):
            ssd_chunk(18 + ci, None, 1, xs_tm_c[:, ci, :], ("xs_tm_c", 0), B_tm_c[:, ci, :], ("B_tm_c", 0), None,
                      hCs[1], "hC1", None, None, cumC[:, 8:16], "cumC", False)
        for n in range(NT):
            ssd_chunk(n + 1, n, 0, xs_tm[:, n, :], ("xs_tm", n), B_tm[:, n, :], ("B_tm", n), slice(n * 128, (n + 1) * 128),
                      hTs[0], "hT0", hTbs[0], "hTb0", cums[0], "cum0", True)
        for n in reversed(range(NT)):
            ssd_chunk(n + 1, n, 1, xs_tm[:, n, :], ("xs_tm", n), B_tm[:, n, :], ("B_tm", n), slice(n * 128, (n + 1) * 128),
                      hTs[1], "hT1", hTbs[1], "hTb1", cums[1], "cum1", True)
        tap("ST", ST, ["hT0", "hT1", "cum0", "cum1"])
        tap("hC", hC_all, ["hC0", "hC1"])

        dma("gpsimd", "xin", xin[:, :], ST, r=["hT0", "hT1", "cum0", "cum1"], w=["xin"])
        S.op("gpsimd", lambda e: e.collective_compute("AllGather", ALU.bypass, replica_groups=[[0, 1, 2, 3], [4, 5, 6, 7]],
                                                      ins=[xin.ap().opt()], outs=[xout.ap().opt()]),
             ["xin"], ["xout"], dsem="C:cc")
        R_hx2 = Region(86 + e_base / 256.0, 40 - e_base / 256.0)
        G_f = R_hx2.get(4 * 1040, name="G")
        G3 = v3(G_f, 4)
        hin = R_hx2.get(1024, name="hin")
        hinb = R_hx2.get(1024, BF16, name="hinb")
        Dall = R_hx2.get(64, name="Dall")
        al = R_hx2.get(8, name="al")
        keys("tmpy2", 2)
        tmpy2 = [R_hx2.get(512, name="tmpy2") for _ in range(2)]
        dma("gpsimd", "G", G3, xout.ap().rearrange("(r p) f -> p r f", p=128), r=["xout"], w=["G"])
        Dall3 = v3(Dall, 4)
        act(Dall3, G3[:, :, 1024:1040], AF.Exp, r=["G"], w=["Dall"])
        for d in range(2):
            hd = hin[:, d * 512:(d + 1) * 512]
            hk = "hin%d" % d
            cp("vector", hd, hCs[d], r=["hC%d" % d], w=[hk])
            order = range(4) if d == 0 else reversed(range(4))
            for i in order:
                mcol = mv[:, 4 * d + i:4 * d + i + 1]
                ts("vector", al, Dall3[:, i, 8 * d:8 * d + 8], -1.0, mcol, ALU.add, ALU.mult, r=["Dall", "mv"], w=["al"])
                ts("vector", al, al, 1.0, None, ALU.add, r=["al"], w=["al"])
                tt("vector", v3(hd, 8), v3(hd, 8), bc(al.unsqueeze(2), [128, 8, 64]), ALU.mult, r=[hk, "al"], w=[hk])
                stt(hd, G3[:, i, d * 512:(d + 1) * 512], mcol, hd, ALU.mult, ALU.add, r=["G", "mv", hk], w=[hk])
        cp("scalar", hinb, hin, r=["hin0", "hin1"], w=["hinb"])
        tap("hin", hin, ["hin0", "hin1"])
        bcnt = [0]
        for n in range(NT):
            for d in range(2):
                bank = bcnt[0] % 4
                sl = bcnt[0] % 2
                bcnt[0] += 1
                for g in range(2):
                    mm(PB(bank)[:, g * 256:(g + 1) * 256], CT[:, g, n * 128:(n + 1) * 128],
                       hinb[:, d * 512 + g * 256:d * 512 + (g + 1) * 256], True, True, r=[("CT", g), "hinb"], w=[("ps", bank)])
                tt("vector", v3(tmpy2[sl], 8), v3(PB(bank), 8), bc(Epass[:, n, d, :].unsqueeze(2), [128, 8, 64]), ALU.mult,
                   r=[("ps", bank), ("Ep", n, d)], w=[("tmpy2", sl)])
                tt("gpsimd", yacc[:, n, :], yacc[:, n, :], tmpy2[sl], ALU.add, r=[("tmpy2", sl), ("yacc", n)], w=[("yacc", n)])
        tap("yacc", yacc_all, keys("yacc", 16))
        tap("CTe", CT.rearrange("p a b -> p (a b)"), keys("CT", 2))
        tap("BTe", BT.rearrange("p a b -> p (a b)"), keys("BT", 2))
        if stop == "E":
            finish()
            return nc

        R_x2 = Region(154, 44)
        keys("woutb", 8)
        woutb = v3(R_x2.get(8 * 1024, BF16, name="woutb"), 8)
        keys("wof", 2)
        wof = [R_x2.get(1024, name="wof") for _ in range(2)]
        for nm_ in ("yg", "mixb", "mixT", "xres", "tmpF"):
            keys(nm_, 2)
        yg = [R_x2.get(512, name="yg") for _ in range(2)]
        mixb = [R_x2.get(512, BF16, name="mixb") for _ in range(2)]
        mixT = [R_x2.get(1024, BF16, name="mixT") for _ in range(2)]
        xres = [R_x2.get(1024, name="xres") for _ in range(2)]
        R_hx3 = Region(86, 40)
        keys("h2b", 16)
        h2b = v3(R_hx3.get(16 * 1024, BF16, name="h2b"), 16)
        h2f = R_hx3.get(1024, name="h2f")
        h2Tf = R_hx3.get(1024, name="h2Tf")
        R_m1c = Region(138, 8)
        keys("lg", 16)
        lg_f = R_m1c.get(16 * 36, name="lg")
        lg = v3(lg_f, 16)
        tmpF = [R_m1c.get(512, name="tmpF") for _ in range(2)]
        st_f = R_misc.get(64)
        st = v3(st_f, 16)
        x1A = yacc
        x1B = v3(siluz_all, 16)
        x1C = v3(attn_all, 16)
        w_out_v = w_out.rearrange("(kc p) n -> p kc n", p=128)
        for kc in range(8):
            sf = kc % 2
            dma("sync", ("wof", sf), wof[sf], w_out_v[:, kc, :], w=[("wof", sf)])
            cp("gpsimd", woutb[:, kc, :], wof[sf], r=[("wof", sf)], w=[("woutb", kc)])
        wr3 = v3(wr, 8)
        g1k = [("g1", 0), ("g1", 1)]
        for n in range(NT):
            sl = n % 2
            tt("vector", yg[sl], yacc[:, n, :], siluz[:, n, :], ALU.mult, r=[("yacc", n), ("siluz", n)], w=[("yg", sl)])
            act(mixb[sl], yg[sl], AF.Square, r=[("yg", sl)], w=[("mixb", sl), ("st", n)], accum=st[:, n, 0:1])
            act(st[:, n, 0:1], st[:, n, 0:1], AF.Sqrt, r=[("st", n)], w=[("st", n)], bias=EPS, scale=1.0 / 512)
            recip(st[:, n, 0:1], st[:, n, 0:1], r=[("st", n)], w=[("st", n)])
            stt(mixb[sl], yg[sl], st[:, n, 0:1], ssdn, ALU.mult, ALU.mult, r=[("yg", sl), ("st", n), "ssdn"], w=[("mixb", sl)])
            bank = n % 2
            for kc in range(4):
                tr(PBh(bank)[:, kc * 128:(kc + 1) * 128], mixb[sl][:, kc * 128:(kc + 1) * 128], identb,
                   r=[("mixb", sl), "identb"], w=[("ps", bank)])
            for kc in range(4):
                tr(PBh(bank)[:, (4 + kc) * 128:(5 + kc) * 128], attn[:, n, kc * 128:(kc + 1) * 128], identb,
                   r=[("attn", n), "identb"], w=[("ps", bank)])
            cp("scalar", v3(mixT[sl], 8), v3(PBh(bank)[:, 0:1024], 8), r=[("ps", bank)], w=[("mixT", sl)])
            dma("sync", ("xres", sl), xres[sl], xe[(n + 1) * 128:(n + 2) * 128, :], w=[("xres", sl)])
            mixT3 = v3(mixT[sl], 8)
            for half in range(2):
                b2 = 2 + 2 * (n % 2) + half
                for kc in range(8):
                    mm(PB(b2), mixT3[:, kc, :], woutb[:, kc, half * 512:(half + 1) * 512], kc == 0, kc == 7,
                       r=[("mixT", sl), ("woutb", kc)], w=[("ps", b2)])
                tt("vector", tmpF[half], PB(b2), g1[:, half * 512:(half + 1) * 512], ALU.mult,
                   r=[("ps", b2)] + g1k, w=[("tmpF", half)])
                if half == 0:
                    tt("gpsimd", x1A[:, n, :], tmpF[0], xres[sl][:, 0:512], ALU.add, r=[("tmpF", 0), ("xres", sl)], w=[("yacc", n)])
                else:
                    tt("gpsimd", x1B[:, n, :], tmpF[1][:, 0:256], xres[sl][:, 512:768], ALU.add,
                       r=[("tmpF", 1), ("xres", sl)], w=[("siluz", n)])
                    tt("gpsimd", x1C[:, n, :], tmpF[1][:, 256:512], xres[sl][:, 768:1024], ALU.add,
                       r=[("tmpF", 1), ("xres", sl)], w=[("attn", n)])
            pieces = ((x1A[:, n, :], ("yacc", n), 0, 512), (x1B[:, n, :], ("siluz", n), 512, 768), (x1C[:, n, :], ("attn", n), 768, 1024))
            for pi, (xp, xk, c0, c1) in enumerate(pieces):
                act(h2f[:, c0:c1], xp, AF.Square, r=[xk], w=["h2f", ("st", n)], accum=st[:, n, 1 + pi:2 + pi])
            tt("vector", st[:, n, 1:2], st[:, n, 1:2], st[:, n, 2:3], ALU.add, r=[("st", n)], w=[("st", n)])
            tt("vector", st[:, n, 1:2], st[:, n, 1:2], st[:, n, 3:4], ALU.add, r=[("st", n)], w=[("st", n)])
            act(st[:, n, 1:2], st[:, n, 1:2], AF.Sqrt, r=[("st", n)], w=[("st", n)], bias=EPS, scale=1.0 / D)
            recip(st[:, n, 1:2], st[:, n, 1:2], r=[("st", n)], w=[("st", n)])
            for xp, xk, c0, c1 in pieces:
                stt(h2f[:, c0:c1], xp, st[:, n, 1:2], gsc2[:, c0:c1], ALU.mult, ALU.mult,
                    r=[xk, ("st", n), ("gsc2", 0), ("gsc2", 1)], w=["h2f"])
            tt("gpsimd", h2f, h2f, sh2, ALU.add, r=["h2f", ("sh2", 0), ("sh2", 1)], w=["h2f"])
            cp("scalar", h2b[:, n, :], h2f, r=["h2f"], w=[("h2b", n)])
            for kc in range(8):
                tr(PB(6 + kc // 4)[:, (kc % 4) * 128:(kc % 4 + 1) * 128], h2f[:, kc * 128:(kc + 1) * 128], identf,
                   r=["h2f", "identf"], w=[("ps", 6 + kc // 4)])
            h2Tf3 = v3(h2Tf, 8)
            for hb in range(2):
                cp("vector", h2Tf3[:, 4 * hb:4 * hb + 4, :], v3(PB(6 + hb), 4), r=[("ps", 6 + hb)], w=["h2Tf"])
            rb = 2 + 2 * (n % 2)
            for kc in range(8):
                mm(PB(rb)[:, 0:36], h2Tf3[:, kc, :], wr3[:, kc, :], kc == 0, kc == 7, r=["h2Tf", "wr"], w=[("ps", rb)])
            tt("vector", lg[:, n, :], PB(rb)[:, 0:36], brb, ALU.add, r=[("ps", rb), "brb"], w=[("lg", n)])
        tap("x1a", yacc_all, keys("yacc", 16))
        tap("x1b", siluz_all, keys("siluz", 16))
        tap("x1c", attn_all, keys("attn", 16))
        tap("lg", lg_f, keys("lg", 16))

        R_x3 = Region(154, 44)
        S.keyname["tk"] = "tkall"
        S.alias["tkall"] = [k_ for nm_ in ("BT", "CT", "BTc", "xs_tm", "B_tm", "xs_tm_c", "B_tm_c", "woutb", "wof", "yg", "mixb", "mixT", "xres") for k_ in ALLKEYS.get(nm_, [nm_])]
        tk = lambda nelem, nm: R_x3.get(nelem, name=nm)
        gmax, gsum, gw, m1, m2, w2, dn2, c1, c2 = [tk(16, "tk%d" % i) for i in range(9)]
        gsel, gexp, gselm = [v3(tk(64, "tk1%d" % i), 16) for i in range(3)]
        lm, sel1, lm2, sel2 = [v3(tk(512, "tk2%d" % i), 16) for i in range(4)]
        lgG, lgE = lg[:, :, 0:4], lg[:, :, 4:36]
        KL = keys("lg", 16)
        BIG = 30000.0
        b4 = lambda a: bc(a.unsqueeze(2), [128, 16, 4])
        b32 = lambda a: bc(a.unsqueeze(2), [128, 16, 32])
        red(gmax, lgG, ALU.max, r=KL, w=["tk"])
        tt("vector", gsel, lgG, b4(gmax), ALU.is_equal, r=KL + ["tk"], w=["tk"])
        tt("vector", gexp, lgG, b4(gmax), ALU.subtract, r=KL + ["tk"], w=["tk"])
        act(gexp, gexp, AF.Exp, r=["tk"], w=["tk"])
        red(gsum, gexp, ALU.add, r=["tk"], w=["tk"])
        recip(gw, gsum, r=["tk"], w=["tk"])
        ts("vector", gselm, gsel, -1.0, BIG, ALU.add, ALU.mult, r=["tk"], w=["tk"])
        lm4 = lm.rearrange("p n (g j) -> p n g j", g=4)
        lgE4 = lgE.rearrange("p n (g j) -> p n g j", g=4)
        tt("vector", lm4, lgE4, bc(gselm.unsqueeze(3), [128, 16, 4, 8]), ALU.add, r=KL + ["tk"], w=["tk"])
        red(m1, lm, ALU.max, r=["tk"], w=["tk"])
        tt("vector", sel1, lm, b32(m1), ALU.is_equal, r=["tk"], w=["tk"])
        stt(lm2, sel1, -BIG, lm, ALU.mult, ALU.add, r=["tk"], w=["tk"])
        red(m2, lm2, ALU.max, r=["tk"], w=["tk"])
        tt("vector", sel2, lm2, b32(m2), ALU.is_equal, r=["tk"], w=["tk"])
        tt("vector", w2, m2, m1, ALU.subtract, r=["tk"], w=["tk"])
        act(w2, w2, AF.Exp, r=["tk"], w=["tk"])
        ts("vector", dn2, w2, 1.0, None, ALU.add, r=["tk"], w=["tk"])
        recip(dn2, dn2, r=["tk"], w=["tk"])
        tt("vector", c1, gw, dn2, ALU.mult, r=["tk"], w=["tk"])
        tt("vector", c2, c1, w2, ALU.mult, r=["tk"], w=["tk"])
        Mm_f = tk(512, "tk30")
        Mm = v3(Mm_f, 16)
        tt("vector", Mm, sel1, sel2, ALU.add, r=["tk"], w=["tk"])
        for i in range(9):
            ALLKEYS["tk%d" % i] = ["tk"]
        for i in range(3):
            ALLKEYS["tk1%d" % i] = ["tk"]
        for i in range(4):
            ALLKEYS["tk2%d" % i] = ["tk"]
        if stop == "F":
            finish()
            return nc

        I32 = mybir.dt.int32
        BIGI = float(NE * CAP)
        eCi = tk(32, "tk40").bitcast(I32)
        eC = tk(32, "tk41")
        Mcum_f = tk(512, "tk42")
        Mcum = v3(Mcum_f, 16)
        slot = v3(tk(512, "tk43"), 16)
        okm = v3(tk(512, "tk44"), 16)
        tmpk = v3(tk(512, "tk45"), 16)
        dfl = tk(32, "tk46")
        wsel = R_misc.get(96)
        wc = [wsel[:, 0:16], wsel[:, 16:32]]
        di = [wsel[:, 32:48].bitcast(I32), wsel[:, 48:64].bitcast(I32)]
        dg = [wsel[:, 64:80].bitcast(I32), wsel[:, 80:96].bitcast(I32)]
        for i in range(40, 47):
            ALLKEYS["tk%d" % i] = ["tk"]
        ALLKEYS["tk30"] = ["tk"]
        S.op("gpsimd", lambda e: e.iota(out=eCi, pattern=[[CAP, 32]], base=0, channel_multiplier=0), (), ["eCi"])
        cp("vector", eC, eCi, r=["eCi"], w=["tk"])
        mset("vector", Mcum[:, 0, :], 0.0, w=["tk"])
        for n in range(1, NT):
            tt("vector", Mcum[:, n, :], Mcum[:, n - 1, :], Mm[:, n - 1, :], ALU.add, r=["tk"], w=["tk"])
        mm(PB(0), LT, Mm_f, True, False, r=["LT", "tk"], w=[("ps", 0)])
        mm(PB(0), onesf, Mcum_f, False, True, r=["onesf", "tk"], w=[("ps", 0)])
        ts("vector", okm, v3(PB(0), 16), float(CAP), None, ALU.is_lt, r=[("ps", 0)], w=["tk"])
        tt("vector", slot, v3(PB(0), 16), bc(eC.unsqueeze(1), [128, 16, 32]), ALU.add, r=[("ps", 0), "tk"], w=["tk"])
        ts("vector", slot, slot, -BIGI, None, ALU.add, r=["tk"], w=["tk"])
        tt("vector", slot, slot, okm, ALU.mult, r=["tk"], w=["tk"])
        ts("vector", slot, slot, BIGI, None, ALU.add, r=["tk"], w=["tk"])
        for k, selk in ((0, sel1), (1, sel2)):
            tt("vector", tmpk, selk, slot, ALU.mult, r=["tk"], w=["tk"])
            red(dfl[:, 16 * k:16 * k + 16], tmpk, ALU.add, r=["tk"], w=["tk"])
        cp("vector", di[0], dfl[:, 0:16], r=["tk"], w=["wsel"])
        cp("vector", di[1], dfl[:, 16:32], r=["tk"], w=["wsel"])
        ts("vector", dfl, dfl, BIGI - 1.0, None, ALU.min, r=["tk"], w=["tk"])
        cp("vector", dg[0], dfl[:, 0:16], r=["tk"], w=["wsel"])
        cp("vector", dg[1], dfl[:, 16:32], r=["tk"], w=["wsel"])
        cp("vector", wc[0], c1, r=["tk"], w=["wsel"])
        cp("vector", wc[1], c2, r=["tk"], w=["wsel"])
        tap("dfl", dfl, ["tk"])
        tap("wsel", wsel, ["wsel"])
        tap("Mm", Mm_f, ["tk"])
        if stop == "G":
            finish()
            return nc
        for n in range(NT):
            for k in range(2):
                S.op("gpsimd", lambda e, n=n, k=k: e.indirect_dma_start(
                    out=Xbuf[:, :], out_offset=bass.IndirectOffsetOnAxis(ap=di[k][:, n:n + 1], axis=0),
                    in_=h2b[:, n, :], in_offset=None, bounds_check=NE * CAP - 1, oob_is_err=False),
                    ["wsel", ("h2b", n), "Xzero"], ["Xbuf"], dsem="G:scat")

        R_w1 = Region(134, 12)
        R_w2 = Region(154, 44)
        for nm_ in ("wgb", "wub", "wdb"):
            keys(nm_, 2)
        wgb = [v3(R_w2.get(8 * 512, BF16, name="wgb"), 8) for _ in range(2)]
        wub = [v3(R_w2.get(8 * 512, BF16, name="wub"), 8) for _ in range(2)]
        wdb = [v3(R_w2.get(4 * 1024, BF16, name="wdb"), 4), v3(R_w1.get(4 * 1024, BF16, name="wdb"), 4)]
        keys("sg", 2)
        sg = [R_w2.get(512, BF16, name="sg") for _ in range(2)]
        R_g = Region(86, 32)
        keys("Xg", 2)
        keys("xgT", 2)
        Xg = [v3(R_g.get(4 * 1024, BF16, name="Xg"), 4) for _ in range(2)]
        xgT = [v3(R_g.get(8 * 512, BF16, name="xgT"), 8) for _ in range(2)]
        R_hx4 = Region(86 + 32, 8)
        ybf = R_hx4.get(4 * 1024, BF16, name="yb")
        yb = v3(ybf, 4)
        R_m1d = Region(126, 8)
        keys("actT", 2)
        actT = [v3(R_m1d.get(4 * 512, BF16, name="actT"), 4) for _ in range(2)]
        ALLKEYS["h2f"] = ["h2f"]
        ALLKEYS["h2Tf"] = ["h2Tf"]
        g2k = [("g2", 0), ("g2", 1)]
        mcnt = [0]
        pcnt = [0]
        dcnt = [0]
        ceng = ["gpsimd", "vector", "gpsimd", "scalar"]
        n_exp = NE if stop is None else 2

        def load_x(e_):
            dma("sync", ("Xg", e_ % 2), Xg[e_ % 2], Xbuf[e_ * CAP:(e_ + 1) * CAP, :].rearrange("(t p) d -> p t d", p=128),
                r=["Xbuf"], w=[("Xg", e_ % 2)])

        for e in range(n_exp):
            wb = e % 2
            xs_ = e % 2
            wg_v = w_gate[e % ne_decl].rearrange("(kc p) f -> p kc f", p=128)
            wu_v = w_up[e % ne_decl].rearrange("(kc p) f -> p kc f", p=128)
            wd_v = w_down[e % ne_decl].rearrange("(fc p) n -> p fc n", p=128)
            if e == 0:
                load_x(0)
            if e + 1 < n_exp:
                load_x(e + 1)
            dma("gpsimd", ("wgb", wb), wgb[wb], wg_v, w=[("wgb", wb)])
            dma("gpsimd", ("wub", wb), wub[wb], wu_v, w=[("wub", wb)])
            dma("gpsimd", ("wdb", wb), wdb[wb], wd_v, w=[("wdb", wb)])
            for ti in range(4):
                bank = 6 + ti % 2
                for kc in range(8):
                    tr(PBh(bank)[:, kc * 128:(kc + 1) * 128], Xg[xs_][:, ti, kc * 128:(kc + 1) * 128], identb,
                       r=[("Xg", xs_), "identb"], w=[("ps", bank)])
                cp("scalar", xgT[xs_][:, :, ti * 128:(ti + 1) * 128], v3(PBh(bank)[:, 0:1024], 8), r=[("ps", bank)], w=[("xgT", xs_)])
            asl = e % 2
            for fc in range(4):
                pi = pcnt[0] % 2
                pcnt[0] += 1
                bg, bu = 2 * pi, 2 * pi + 1
                for kc in range(8):
                    mm(PB(bg), wgb[wb][:, kc, fc * 128:(fc + 1) * 128], xgT[xs_][:, kc, :], kc == 0, kc == 7,
                       r=[("xgT", xs_), ("wgb", wb)], w=[("ps", bg)])
                for kc in range(8):
                    mm(PB(bu), wub[wb][:, kc, fc * 128:(fc + 1) * 128], xgT[xs_][:, kc, :], kc == 0, kc == 7,
                       r=[("xgT", xs_), ("wub", wb)], w=[("ps", bu)])
                act(sg[pi], PB(bg), AF.Silu, r=[("ps", bg)], w=[("sg", pi)])
                tt("vector", actT[asl][:, fc, :], sg[pi], PB(bu), ALU.mult, r=[("sg", pi), ("ps", bu)], w=[("actT", asl)])
            for ti in range(4):
                for half in range(2):
                    bank = 4 + dcnt[0] % 2
                    dcnt[0] += 1
                    for fc in range(4):
                        mm(PB(bank), actT[asl][:, fc, ti * 128:(ti + 1) * 128], wdb[wb][:, fc, half * 512:(half + 1) * 512],
                           fc == 0, fc == 3, r=[("actT", asl), ("wdb", wb)], w=[("ps", bank)])
                    cp("scalar" if half == 0 else "vector", yb[:, ti, half * 512:(half + 1) * 512], PB(bank), r=[("ps", bank)], w=["yb"])
            dma("sync", "yst", Ybuf[e * CAP:(e + 1) * CAP, :].rearrange("(t p) d -> p t d", p=128), yb, r=["yb"], w=["Ybuf"])

        R_g2 = Region(86, 32)
        keys("yg", 8)
        ygb = [R_g2.get(1024, BF16, name="yg") for _ in range(8)]
        keys("tmpg", 4)
        tmpg = [R_g2.get(1024, name="tmpg") for _ in range(4)]
        for i_ in range(8):
            mset("gpsimd", ygb[i_], 0.0, w=[("yg", i_)])
        R_fin = Region(134, 12)
        nfb = R_fin.get(D, name="nfb")
        keys("ot", 2)
        ot = [R_fin.get(D, name="ot") for _ in range(2)]
        st2_f = R_hx4.get(64, name="st2") if False else R_misc.get(64)
        st2 = v3(st2_f, 16)
        dma("sync", "G:nfb", nfb, normf.partition_broadcast(128), w=["nfb"])
        def final_tile(n):
            sl = n % 2
            pieces = ((x1A[:, n, :], ("yacc", n), 0, 512), (x1B[:, n, :], ("siluz", n), 512, 768), (x1C[:, n, :], ("attn", n), 768, 1024))
            for pi, (xp, xk, c0, c1_) in enumerate(pieces):
                act(ot[sl][:, c0:c1_], xp, AF.Square, r=[xk], w=[("ot", sl), ("st2", n)], accum=st2[:, n, pi:pi + 1])
            tt("vector", st2[:, n, 0:1], st2[:, n, 0:1], st2[:, n, 1:2], ALU.add, r=[("st2", n)], w=[("st2", n)])
            tt("vector", st2[:, n, 0:1], st2[:, n, 0:1], st2[:, n, 2:3], ALU.add, r=[("st2", n)], w=[("st2", n)])
            act(st2[:, n, 0:1], st2[:, n, 0:1], AF.Sqrt, r=[("st2", n)], w=[("st2", n)], bias=EPS, scale=1.0 / D)
            recip(st2[:, n, 0:1], st2[:, n, 0:1], r=[("st2", n)], w=[("st2", n)])
            for xp, xk, c0, c1_ in pieces:
                stt(ot[sl][:, c0:c1_], xp, st2[:, n, 0:1], nfb[:, c0:c1_], ALU.mult, ALU.mult,
                    r=[xk, ("st2", n), "nfb"], w=[("ot", sl)])
            dma("sync", ("ot", sl), out[n * 128:(n + 1) * 128, :], ot[sl], r=[("ot", sl)], w=[("out", n)])
        for n in range(NT):
            for k in range(2):
                gi_ = 2 * (n % 4) + k
                S.op("gpsimd", lambda e, n=n, k=k, gi_=gi_: e.indirect_dma_start(
                    out=ygb[gi_], out_offset=None, in_=Ybuf[:, :],
                    in_offset=bass.IndirectOffsetOnAxis(ap=dg[k][:, n:n + 1], axis=0)),
                    ["wsel", "Ybuf"], [("yg", gi_)], dsem=("ygd", gi_))
                wcol = wc[k][:, n:n + 1]
                tj = gi_ % 4
                tt("vector" if k == 0 else "gpsimd", tmpg[tj], ygb[gi_], g2, ALU.mult, r=[("yg", gi_)] + g2k, w=[("tmpg", tj)])
                stt(x1A[:, n, :], tmpg[tj][:, 0:512], wcol, x1A[:, n, :], ALU.mult, ALU.add,
                    r=[("tmpg", tj), "wsel", ("yacc", n)], w=[("yacc", n)])
                stt(x1B[:, n, :], tmpg[tj][:, 512:768], wcol, x1B[:, n, :], ALU.mult, ALU.add,
                    r=[("tmpg", tj), "wsel", ("siluz", n)], w=[("siluz", n)])
                stt(x1C[:, n, :], tmpg[tj][:, 768:1024], wcol, x1C[:, n, :], ALU.mult, ALU.add,
                    r=[("tmpg", tj), "wsel", ("attn", n)], w=[("attn", n)])
            if n >= 1:
                final_tile(n - 1)
        final_tile(NT - 1)
        finish()
        return nc
def _rope_tables():
    pos = np.arange(8192)
    row, col = pos // 64, pos % 64
    inv = 10000.0 ** (-np.arange(16, dtype=np.float64) / 16)
    ang = np.concatenate([row[:, None] * inv, col[:, None] * inv], axis=-1)
    ang = np.concatenate([ang, ang], axis=-1)
    cos, sin = np.cos(ang), np.sin(ang)
    sgn = np.where(np.arange(64) < 32, -1.0, 1.0)
    return cos.T.astype(np.float32), (sin * sgn).T.astype(np.float32)


def _fm(w, cols):
    wc = w[:, cols]
    n = wc.shape[1]
    return np.ascontiguousarray(wc.reshape(8, 128, n).transpose(1, 0, 2)).reshape(128, 8 * n)


def prep_inputs(inputs):
    f = lambda k: np.asarray(inputs[k], np.float32)
    x, c, ctx, c_ctx = f("x"), f("c"), f("ctx"), f("c_ctx")
    w_in = f("w_in")[0]
    ar = np.arange
    chunks = [512 + 128 * j + ar(128) for j in range(8)]
    chunks += [1552 + 128 * cc + ar(128) for cc in range(4)]
    perm = (ar(64) + 32) % 64
    chunks += [1552 + 128 * cc + np.concatenate([perm, 64 + perm]) for cc in range(4)]
    chunks += [2064 + 64 * hk + np.concatenate([ar(64), ar(64)]) for hk in range(2)]
    chunks += [2064 + 64 * hk + np.concatenate([perm, perm]) for hk in range(2)]
    wfm = np.stack([_fm(w_in, cc) for cc in chunks])
    wvd = _fm(w_in, np.concatenate([2192 + ar(128), 1536 + ar(16)]))
    wz = _fm(w_in, ar(512))
    convw = np.ascontiguousarray(f("conv_w")[0].T.reshape(8, 128, 5).transpose(1, 0, 2)).reshape(128, 40)
    convb = np.ascontiguousarray(f("conv_b")[0].reshape(8, 128).T)
    wr = _fm(np.concatenate([f("w_group")[0], f("w_expert")[0]], axis=1), ar(36))
    br = np.concatenate([f("b_group")[0], f("b_expert")[0]])
    cosT, sinT = _rope_tables()
    jj, ii = np.meshgrid(ar(128), ar(128), indexing="ij")
    m_prev = np.where(jj >= ii, 0.0, NEG).astype(np.float32)
    m_next = np.where(jj <= ii, 0.0, NEG).astype(np.float32)
    m_none = np.full((128, 128), NEG, np.float32)
    shared = dict(
        wfm=wfm, wvd=wvd, wz=wz, w_ada=f("w_ada")[0], b_ada=f("b_ada")[0], norm1=f("norm1")[0],
        convw=convw, convb=convb, dtb=f("dt_bias")[0].reshape(16), alog=f("a_log")[0].reshape(16),
        dskip=f("d_skip")[0], ssdnorm=f("ssd_norm")[0], sinks=f("attn_sinks")[0], w_out=f("w_out")[0],
        norm2=f("norm2")[0], wr=wr, br=br, w_gate=f("w_gate")[0], w_up=f("w_up")[0], w_down=f("w_down")[0],
        normf=f("norm_final"))
    maps = []
    for r in range(NCORES):
        b, s = r // 4, r % 4
        t0 = s * T
        xe = np.zeros((2304, D), np.float32)
        lo, hi = max(t0 - 128, 0), min(t0 + 2176, 8192)
        xe[lo - (t0 - 128):hi - (t0 - 128)] = x[b, lo:hi]
        rope = np.zeros((128, 2 * 2304), np.float32)
        for half in range(2):
            rope[half * 64:(half + 1) * 64, lo - (t0 - 128):hi - (t0 - 128)] = cosT[:, lo:hi]
            rope[half * 64:(half + 1) * 64, 2304 + lo - (t0 - 128):2304 + hi - (t0 - 128)] = sinT[:, lo:hi]
        cfm = np.zeros((128, 16), np.float32)
        cfm[:, 0:8] = c[b].reshape(8, 128).T
        cfm[:, 8:16] = c_ctx.reshape(8, 128).T
        vmask = np.zeros((128, 4), np.float32)
        vmask[:, 0] = 1.0 if s > 0 else 0.0
        vmask[:, 1] = 1.0 if s < 3 else 0.0
        mvec = np.zeros((128, 8), np.float32)
        for i in range(4):
            mvec[:, i] = 1.0 if i < s else 0.0
            mvec[:, 4 + i] = 1.0 if i > s else 0.0
        am = np.stack([m_prev, m_next, m_prev if s > 0 else m_none, m_next if s < 3 else m_none], axis=1)
        am = np.repeat(am[:, :, None, :], 4, axis=2).reshape(128, 4 * 512)
        m = dict(shared)
        m.update(xe=xe, ctxb=np.ascontiguousarray(ctx[b]), cfm=cfm, vmask=vmask, mvec=mvec,
                 amask=am.astype(ml_dtypes.bfloat16), rope=rope.astype(ml_dtypes.bfloat16))
        maps.append(m)
    return maps


_NC_CACHE = {}


def kernel(**inputs):
    maps = prep_inputs(inputs)
    if "nc" not in _NC_CACHE:
        _NC_CACHE["nc"] = build_nc()
    res = run_bass_kernel_spmd(_NC_CACHE["nc"], maps, core_ids=list(range(NCORES)))
    outp = np.zeros((2, 8192, D), np.float32)
    for r in range(NCORES):
        b, s = r // 4, r % 4
        outp[b, s * T:(s + 1) * T] = res.results[r]["out"]
    return outp
```
